# Optimizing a Trainium2 kernel written in Bass

```python
import math
import jax, jax.numpy as jnp
from jax import lax
import numpy as np

D_MODEL = 1024
BATCH = 8
SEQ = 2048
DEPTH = 2

CHUNK = 64
D_MIX = D_MODEL
CONV_GROUPS = 8
CONV_GROUP_DIM = 64
W_CONV = CONV_GROUPS * CONV_GROUP_DIM
CONV_K = 3
LRU_HEADS = 8
LRU_HEAD_DIM = 64
W_LRU = LRU_HEADS * LRU_HEAD_DIM
LRU_CONV_K = 4
RG_C = 8.0
D_IN_TOT = 3 * W_CONV + 2 * W_LRU
D_FF = 3584
N_EXPERTS = 8
TOP_K = 2
N_DENSE = (DEPTH + 1) // 2
N_MOE = DEPTH // 2
EPS = 1e-6

kernel_name = "hybrid_shortconv_rglru_moe_trunk"


def rms_norm(x, g):
    xf = x.astype(jnp.float32)
    y = xf * lax.rsqrt(jnp.mean(xf * xf, axis=-1, keepdims=True) + EPS)
    return (y * g.astype(jnp.float32)).astype(x.dtype)


def causal_depthwise_conv(x, w):
    k = w.shape[0]
    c = x.shape[-1]
    return lax.conv_general_dilated(
        x, w[:, None, :].astype(x.dtype), window_strides=(1,), padding=[(k - 1, 0)],
        dimension_numbers=("NWC", "WIO", "NWC"), feature_group_count=c)


def chunked_linear_scan(a, b):
    bsz, s, w = a.shape
    nc = s // CHUNK
    a_c = a.reshape(bsz, nc, CHUNK, w).transpose(1, 0, 2, 3)
    b_c = b.reshape(bsz, nc, CHUNK, w).transpose(1, 0, 2, 3)

    def combine(left, right):
        al, bl = left
        ar, br = right
        return al * ar, ar * bl + br

    def step(h_prev, ab):
        ac, bc = ab
        a_cum, b_cum = lax.associative_scan(combine, (ac, bc), axis=1)
        h = a_cum * h_prev[:, None, :] + b_cum
        return h[:, -1, :], h

    h0 = jnp.zeros((bsz, w), jnp.float32)
    _, hs = lax.scan(step, h0, (a_c, b_c))
    return hs.transpose(1, 0, 2, 3).reshape(bsz, s, w)


def rg_lru(x, wa, ba, wx, bx, lam):
    bsz, s, _ = x.shape
    xh = x.reshape(bsz, s, LRU_HEADS, LRU_HEAD_DIM)
    r = jax.nn.sigmoid((jnp.einsum("bshi,hij->bshj", xh, wa).reshape(bsz, s, W_LRU) + ba).astype(jnp.float32))
    i = jax.nn.sigmoid((jnp.einsum("bshi,hij->bshj", xh, wx).reshape(bsz, s, W_LRU) + bx).astype(jnp.float32))
    log_a = -RG_C * r * jax.nn.softplus(-lam.astype(jnp.float32))
    a = jnp.exp(log_a)
    b = jnp.sqrt(-jnp.expm1(2.0 * log_a)) * (i * x.astype(jnp.float32))
    return chunked_linear_scan(a, b).astype(x.dtype)


def hybrid_mixer(h, w_in, conv_w, lru_conv_w, lru_conv_b, wa, ba, wx, bx, lam, w_out):
    p = h @ w_in
    c_gate, b_gate, v, u, g = jnp.split(
        p, [W_CONV, 2 * W_CONV, 3 * W_CONV, 3 * W_CONV + W_LRU], axis=-1)
    y_a = b_gate * causal_depthwise_conv(c_gate * v, conv_w)
    uc = causal_depthwise_conv(u, lru_conv_w) + lru_conv_b
    y_b = rg_lru(uc, wa, ba, wx, bx, lam) * jax.nn.gelu(g)
    return jnp.concatenate([y_a, y_b], axis=-1) @ w_out


def swiglu(x, wg, wu, wd):
    return (jax.nn.silu(x @ wg) * (x @ wu)) @ wd


def moe_swiglu(h, w_router, wg, wu, wd):
    bsz, s, d = h.shape
    xt = h.reshape(-1, d)
    logits = (xt @ w_router).astype(jnp.float32)
    top_v, top_i = lax.top_k(logits, TOP_K)
    gates = jax.nn.softmax(top_v, axis=-1)
    comb = jnp.sum(jax.nn.one_hot(top_i, N_EXPERTS, dtype=jnp.float32) * gates[..., None], axis=1)
    comb = comb.astype(h.dtype)
    y = jnp.zeros_like(xt)
    for e in range(N_EXPERTS):
        y = y + comb[:, e:e + 1] * swiglu(xt, wg[e], wu[e], wd[e])
    return y.reshape(bsz, s, d)


def setup_inputs(seed: int = 0) -> dict:
    key = jax.random.key(seed)
    ks = jax.random.split(key, 24)
    f32 = jnp.float32
    nrm = lambda k, shape, fan_in: jax.random.normal(k, shape, f32) * (fan_in ** -0.5)
    u = jax.random.uniform(ks[11], (DEPTH, W_LRU), f32, minval=0.9, maxval=0.999)
    a0 = u ** (1.0 / RG_C)
    lam = jnp.log(a0) - jnp.log1p(-a0)
    return {
        "x": jax.random.normal(ks[0], (BATCH, SEQ, D_MODEL), f32),
        "norm_mix": 1.0 + 0.02 * jax.random.normal(ks[1], (DEPTH, D_MODEL), f32),
        "norm_ffn": 1.0 + 0.02 * jax.random.normal(ks[2], (DEPTH, D_MODEL), f32),
        "norm_final": 1.0 + 0.02 * jax.random.normal(ks[3], (D_MODEL,), f32),
        "w_in": nrm(ks[4], (DEPTH, D_MODEL, D_IN_TOT), D_MODEL),
        "conv_w": nrm(ks[5], (DEPTH, CONV_K, W_CONV), CONV_K),
        "lru_conv_w": nrm(ks[6], (DEPTH, LRU_CONV_K, W_LRU), LRU_CONV_K),
        "lru_conv_b": 0.02 * jax.random.normal(ks[7], (DEPTH, W_LRU), f32),
        "lru_wa": nrm(ks[8], (DEPTH, LRU_HEADS, LRU_HEAD_DIM, LRU_HEAD_DIM), LRU_HEAD_DIM),
        "lru_ba": 0.02 * jax.random.normal(ks[9], (DEPTH, W_LRU), f32),
        "lru_wx": nrm(ks[10], (DEPTH, LRU_HEADS, LRU_HEAD_DIM, LRU_HEAD_DIM), LRU_HEAD_DIM),
        "lru_bx": 0.02 * jax.random.normal(ks[12], (DEPTH, W_LRU), f32),
        "lru_lambda": lam,
        "w_out": nrm(ks[13], (DEPTH, D_MIX, D_MODEL), D_MIX),
        "ffn_w_gate": nrm(ks[14], (N_DENSE, D_MODEL, D_FF), D_MODEL),
        "ffn_w_up": nrm(ks[15], (N_DENSE, D_MODEL, D_FF), D_MODEL),
        "ffn_w_down": nrm(ks[16], (N_DENSE, D_FF, D_MODEL), D_FF),
        "w_router": nrm(ks[17], (N_MOE, D_MODEL, N_EXPERTS), D_MODEL),
        "moe_w_gate": nrm(ks[18], (N_MOE, N_EXPERTS, D_MODEL, D_FF), D_MODEL),
        "moe_w_up": nrm(ks[19], (N_MOE, N_EXPERTS, D_MODEL, D_FF), D_MODEL),
        "moe_w_down": nrm(ks[20], (N_MOE, N_EXPERTS, D_FF, D_MODEL), D_FF),
    }


def reference(x, norm_mix, norm_ffn, norm_final, w_in, conv_w, lru_conv_w, lru_conv_b,
              lru_wa, lru_ba, lru_wx, lru_bx, lru_lambda, w_out,
              ffn_w_gate, ffn_w_up, ffn_w_down,
              w_router, moe_w_gate, moe_w_up, moe_w_down):
    for l in range(DEPTH):
        h = rms_norm(x, norm_mix[l])
        x = x + hybrid_mixer(h, w_in[l], conv_w[l], lru_conv_w[l], lru_conv_b[l],
                             lru_wa[l], lru_ba[l], lru_wx[l], lru_bx[l], lru_lambda[l], w_out[l])
        h = rms_norm(x, norm_ffn[l])
        j = l // 2
        if l % 2 == 0:
            x = x + swiglu(h, ffn_w_gate[j], ffn_w_up[j], ffn_w_down[j])
        else:
            x = x + moe_swiglu(h, w_router[j], moe_w_gate[j], moe_w_up[j], moe_w_down[j])
    return rms_norm(x, norm_final)
```

```python
import numpy as np
from contextlib import ExitStack
import concourse.bass as bass
import concourse.mybir as mybir
from concourse.bass_utils import run_bass_kernel_spmd

F32 = mybir.dt.float32
BF16 = mybir.dt.bfloat16
AF = mybir.ActivationFunctionType
ALU = mybir.AluOpType

D = 1024
S = 2048
NT = 16
NB = 4
DFF = 3584
NG = 7
NE = 8
DIN = 2560
EPS = 1e-6
NRING = 6
GC0 = 0.7978845608028654
GC1 = 0.044715

PV_GMIX = 0
PV_GFFN = 8
PV_CONV = 16
PV_LCW = 28
PV_LCB = 44
PV_BA = 48
PV_BX = 52
PV_LAM = 56
PV_L = 60
PV_GFIN = 2 * PV_L
PV_ID = PV_GFIN + 8
PV_U = PV_ID + 128
PV_TOK = PV_U + 128
PV_IOTA = PV_TOK + 16
PV_N = PV_IOTA + 8
U32 = mybir.dt.uint32
I32 = mybir.dt.int32
JMAX = 5


class Tok:
    __slots__ = ("sem", "val")

    def __init__(self, sem, val):
        self.sem = sem
        self.val = val


class Buf:
    __slots__ = ("name", "w", "r")

    def __init__(self, name):
        self.name = name
        self.w = None
        self.r = {}


class DSem:
    __slots__ = ("h", "count")

    def __init__(self, h):
        self.h = h
        self.count = 0


class Eng:
    def __init__(self, name, sem):
        self.name = name
        self.sem = sem
        self.count = 0
        self.seen = {}
        self.ops = []


class Prog:
    ENGS = (("pe", "tensor"), ("act", "scalar"), ("dve", "vector"), ("pool", "gpsimd"), ("sp", "sync"))

    def __init__(self, nc, st):
        self.nc = nc
        self.st = st
        self.eng = {}
        for n, _ in self.ENGS:
            self.eng[n] = Eng(n, st.enter_context(nc.semaphore("prog_" + n)))
        self.nbuf = 0
        self.guard = None
        self.dsems = []
        self.regs = {}
        for n, attr in self.ENGS:
            eng = getattr(nc, attr)
            self.regs[n] = [st.enter_context(eng.register(f"rg_{n}{i}")) for i in range(2)]

    def buf(self, name="b"):
        self.nbuf += 1
        return Buf(f"{name}{self.nbuf}")

    def dsem(self, name):
        d = DSem(self.st.enter_context(self.nc.semaphore(name)))
        self.dsems.append(d)
        return d

    def regload(self, parity, ap, reads):
        for n, _ in self.ENGS:
            e = self.eng[n]
            waits = self._waits(e, reads, ())
            reg = self.regs[n][parity]
            e.ops.append((waits, [lambda eng, reg=reg: eng.reg_load(reg, ap)], None, None, None))

    def region_begin(self, parity, thr):
        self._saved_seen = {n: dict(e.seen) for n, e in self.eng.items()}
        d0 = {id(d.h): d.count for d in self.dsems}
        for n, _ in self.ENGS:
            self.eng[n].ops.append(("region_begin", parity, thr, d0))

    def region_end(self):
        for n, _ in self.ENGS:
            self.eng[n].ops.append(("region_end",))
            self.eng[n].seen = self._saved_seen[n]

    def barrier(self):
        toks = [Tok(e.sem, e.count) for e in self.eng.values() if e.count > 0]
        toks += [Tok(d.h, d.count) for d in self.dsems if d.count > 0]
        for n, _ in self.ENGS:
            self.wait(n, toks)

    def _waits(self, e, reads, writes, extra=()):
        need = {}

        def want(t):
            if t is None:
                return
            k = id(t.sem)
            if k not in need or need[k].val < t.val:
                need[k] = t
        for b in reads:
            want(b.w)
        for b in writes:
            want(b.w)
            for t in b.r.values():
                want(t)
        for t in extra:
            want(t)
        waits = []
        for k, t in need.items():
            if e.seen.get(k, 0) < t.val:
                e.seen[k] = t.val
                waits.append((t.sem, t.val))
        return waits

    def _commit(self, tok, reads, writes):
        k = id(tok.sem)
        for b in reads:
            if k not in b.r or b.r[k].val < tok.val:
                b.r[k] = tok
        for b in writes:
            b.w = tok
            b.r = {}

    def op(self, en, fn, reads=(), writes=(), dma=None, extra=(), skip=None):
        e = self.eng[en]
        waits = self._waits(e, reads, writes, extra)
        if dma is not None:
            dma.count += 16
            tok = Tok(dma.h, dma.count)
            inc = (dma.h, 16)
        else:
            e.count += 1
            tok = Tok(e.sem, e.count)
            inc = (e.sem, 1)
        e.ops.append((waits, [fn], inc, self.guard, skip))
        self._commit(tok, reads, writes)
        return tok

    def group(self, en, fns, reads=(), writes=(), skip=None):
        e = self.eng[en]
        waits = self._waits(e, reads, writes)
        e.count += 1
        tok = Tok(e.sem, e.count)
        e.ops.append((waits, list(fns), (e.sem, 1), self.guard, skip))
        self._commit(tok, reads, writes)
        return tok

    def wait(self, en, toks):
        e = self.eng[en]
        waits = self._waits(e, (), (), extra=toks)
        e.ops.append((waits, [], None, None, None))

    def emit(self):
        block = self.st.enter_context(self.nc.Block())
        for n, attr in self.ENGS:
            ops = self.eng[n].ops

            regs = self.regs[n]

            def emit_ops(e, ops, regs):
                i = 0
                while i < len(ops):
                    ent = ops[i]
                    if ent[0] == "region_begin":
                        depth, k = 1, i + 1
                        while depth:
                            if ops[k][0] == "region_begin":
                                depth += 1
                            elif ops[k][0] == "region_end":
                                depth -= 1
                            k += 1
                        inner = ops[i + 1:k - 1]
                        totals = {}
                        for o in inner:
                            if len(o) == 5 and o[2] is not None:
                                key = id(o[2][0])
                                totals[key] = (o[2][0], totals.get(key, (None, 0))[1] + o[2][1])
                        if any(len(o) == 5 and o[1] for o in inner):
                            with e.If_lt(regs[ent[1]], ent[2]):
                                for sem, tot in totals.values():
                                    if ent[3].get(id(sem), 0) > 0:
                                        e.wait_ge(sem, ent[3][id(sem)])
                                    while tot > 0:
                                        e.drain().then_inc(sem, min(tot, 4096))
                                        tot -= 4096
                            with e.Else():
                                emit_ops(e, inner, regs)
                        i = k
                        continue
                    waits, fns, inc, guard, skip = ent
                    for sem, val in waits:
                        e.wait_ge(sem, val)
                    i += 1
                    if not fns:
                        continue
                    if guard is None:
                        for fn in fns[:-1]:
                            fn(e)
                        ins = fns[-1](e)
                        if inc is not None:
                            ins.then_inc(inc[0], inc[1])
                    else:
                        par, thr = guard
                        with e.If_lt(regs[par], thr):
                            (skip(e) if skip is not None else e.drain()).then_inc(inc[0], inc[1])
                        with e.Else():
                            for fn in fns[:-1]:
                                fn(e)
                            fns[-1](e).then_inc(inc[0], inc[1])

            def body(e, ops=ops, regs=regs):
                emit_ops(e, ops, regs)
            getattr(block, attr)(body)


def build(layers=(0, 1), final_norm=True, n_experts=NE, ffn_groups=NG, sparse=True):
    nc = bass.Bass("TRN2", target_bir_lowering=False)
    x_d = nc.dram_tensor("x", [S, D], F32, kind="ExternalInput").ap()
    pv_d = nc.dram_tensor("pv", [128, PV_N], F32, kind="ExternalInput").ap()
    gfin_d = nc.dram_tensor("gfin", [128, D], F32, kind="ExternalInput").ap()
    w_in_d = nc.dram_tensor("w_in", [2, D, DIN], F32, kind="ExternalInput").ap()
    w_out_d = nc.dram_tensor("w_out", [2, D, D], F32, kind="ExternalInput").ap()
    wa_d = nc.dram_tensor("lru_wa", [2, 8, 64, 64], F32, kind="ExternalInput").ap()
    wx_d = nc.dram_tensor("lru_wx", [2, 8, 64, 64], F32, kind="ExternalInput").ap()
    if 0 in layers:
        fg_d = nc.dram_tensor("ffn_w_gate", [1, D, DFF], F32, kind="ExternalInput").ap()
        fu_d = nc.dram_tensor("ffn_w_up", [1, D, DFF], F32, kind="ExternalInput").ap()
        fd_d = nc.dram_tensor("ffn_w_down", [1, DFF, D], F32, kind="ExternalInput").ap()
    if 1 in layers:
        wr_d = nc.dram_tensor("w_router", [1, D, NE], F32, kind="ExternalInput").ap()
        mg_d = nc.dram_tensor("moe_w_gate", [1, NE, D, DFF], F32, kind="ExternalInput").ap()
        mu_d = nc.dram_tensor("moe_w_up", [1, NE, D, DFF], F32, kind="ExternalInput").ap()
        md_d = nc.dram_tensor("moe_w_down", [1, NE, DFF, D], F32, kind="ExternalInput").ap()
    out_d = nc.dram_tensor("out", [S, D], F32, kind="ExternalOutput").ap()
    sparse = sparse and (1 in layers)
    if sparse:
        H_d = nc.dram_tensor("h_scr", [S, D], BF16, kind="Internal").ap()
        Xs_d = nc.dram_tensor("xs_scr", [S, D], F32, kind="Internal").ap()
        Y_d = nc.dram_tensor("y_scr", [NE * S, D], F32, kind="Internal").ap()
        T2_d = nc.dram_tensor("t2_scr", [128 * 128, 2], U32, kind="Internal").ap()

    with ExitStack() as st:
        P = Prog(nc, st)

        def sb(name, shape, dt):
            return st.enter_context(nc.sbuf_tensor(name, shape, dt))

        xs = sb("xs", [128, NT, D], F32)
        hT = sb("hT", [128, 8, S], BF16)
        hTm = sb("hTm", [128, 8, 512], BF16)
        yT = sb("yT", [128, 8, 512], BF16)
        ring = [sb(f"ring{i}", [128, 8, 512], BF16) for i in range(NRING)]
        pv = sb("pvs", [128, PV_N], F32)
        ident = sb("ident", [128, 128], BF16)
        wbd = sb("wbd", [128, 8, 128], BF16)
        cc = sb("cc", [128, 16], F32)
        hbias = sb("hbias", [128, 16], F32)
        ss = sb("ss", [128, 16], F32)
        rs = sb("rs", [128, 16], F32)
        cvs = sb("cvs", [128, 514], F32)
        ups_ = [sb(f"ups{i}", [128, 515], F32) for i in range(2)]
        ctail = sb("ctail", [128, 4, 2], F32)
        utail = sb("utail", [128, 4, 3], F32)
        hlast = sb("hlast", [128, 4], F32)
        s32big = sb("s32big", [128, 12, 512], F32)
        s32 = [s32big[:, i, :] for i in range(12)]
        s16 = [sb(f"s16_{i}", [128, 1024], BF16) for i in range(4)]
        hid = [s16[1], s16[2]]
        hidT = [s16[0], s16[3]]
        HIDK = [("xn", 0), ("xn", 1)]
        HIDTK = [[("junk",)], [("ucb", 0), ("ucb", 1)]]
        if 1 in layers:
            xn32 = s32big[:, 0:2, :].rearrange("p a n -> p (a n)")
            xnT32 = s32big[:, 2:4, :].rearrange("p a (c t) -> p (a c) t", t=128)
            ident32 = sb("ident32", [128, 128], F32)
            wr_sb = sb("wr_sb", [128, 8, NE], F32)
            lg = sb("lg", [128, NE], F32)
            ex = sb("ex", [128, NE], F32)
            comb = sb("comb", [128, NT, NE], F32)
            m8 = sb("m8", [128, 8], F32)
            rsum = sb("rsum", [128, 2], F32)
        if sparse:
            msk = sb("msk", [128, NT, NE], F32)
            m1 = sb("m1", [128, NT, NE], F32)
            mskb = sb("mskb", [128, 128], BF16)
            Ubf = sb("Ubf", [128, 128], BF16)
            onesbf = sb("onesbf", [128, 128], BF16)
            nef = sb("nef", [128, NE], F32)
            cnt_i = sb("cnt_i", [128, NE], I32)
            PEG = sb("PEG", [128, 3, 32], F32)
            Ysel = sb("Ysel", [128, 32], U32)
            Tsc = sb("Tsc", [128, 32], U32)
            ti32 = sb("ti32", [128, 2, 32], I32)
            tokid = sb("tokid", [128, 16, 2], U32)
            idx_all = sb("idx_all", [128, 256], U32)
        pst = [st.enter_context(nc.psum_tensor(f"pst{i}", [128, 1024], BF16)) for i in range(2)]
        psf = [st.enter_context(nc.psum_tensor(f"psf{i}", [128, 512], F32)) for i in range(6)]

        B = {}

        def bf(*key):
            if key not in B:
                B[key] = P.buf(str(key))
            return B[key]

        junk_a = sb("junk_a", [128, 2], F32)
        junk_v = sb("junk_v", [128, 2], F32)

        def skip_for(en, out_ap):
            if P.guard is None:
                return None
            if en == "act":
                return lambda e: e.activation(out=junk_a[0:1, 0:1], in_=junk_a[0:1, 1:2], func=AF.Copy)
            if en == "dve":
                return lambda e: e.memset(junk_v[0:1, 0:1], 0.0)
            if en == "pe" and out_ap is not None:
                tgt = out_ap[0:1, 0:1] if out_ap.dtype == F32 else out_ap[0:1, 0:2].bitcast(F32)
                return lambda e: e.matmul(tgt, lhsT=ident[:, 0:1], rhs=ident[:, 0:1], start=True, stop=True)
            return None

        def O(en, meth, reads=(), writes=(), dma=None, **kw):
            return P.op(en, lambda e: getattr(e, meth)(**kw), reads, writes, dma, skip=skip_for(en, None))

        def G(en, calls, reads=(), writes=()):
            return P.group(en, [(lambda e, m=m, kw=kw: getattr(e, m)(**kw)) for m, kw in calls], reads, writes,
                           skip=skip_for(en, calls[0][1].get("out")))

        ring_sem = [P.dsem(f"ring_s{i}") for i in range(NRING)]
        pv_sem, wr_sem, wbd_sem, gf_sem = P.dsem("pv_s"), P.dsem("wr_s"), P.dsem("wbd_s"), P.dsem("gf_s")
        xl_sem = [P.dsem(f"xl{i}") for i in range(NB)]
        out_sem = P.dsem("outs")
        if sparse:
            xsp_sem, y_sem = P.dsem("xsp_s"), P.dsem("y_s")
            h_sem = [P.dsem(f"h_s{i}") for i in range(2)]
            xr_sem = [P.dsem(f"xr_s{i}") for i in range(NT)]
            t2_sem, sc_sem, ix_sem = P.dsem("t2_s"), P.dsem("sc_s"), P.dsem("ix_s")
            g_sem = [P.dsem(f"g_s{i}") for i in range(6)]
            yg_sem = [P.dsem(f"yg_s{i}") for i in range(6)]
        ring_pos = [0]

        def load_w(src_ap, split=False):
            i = ring_pos[0] % NRING
            ring_pos[0] += 1
            dst = ring[i][:].rearrange("p (c h) n -> p c h n", h=2) if split else ring[i][:]
            O("pool", "dma_start", writes=[bf("ring", i)], dma=ring_sem[i], out=dst, in_=src_ap)
            return i

        O("dve", "memset", writes=[bf("junk_a")], ap=junk_a[:], constant=0.0)
        O("dve", "memset", writes=[bf("junk_v")], ap=junk_v[:], constant=0.0)
        O("sp", "dma_start", writes=[bf("pv")], dma=pv_sem, out=pv[:], in_=pv_d)
        for b in range(NB):
            src = x_d[b * 512:(b + 1) * 512, :].rearrange("(i p) d -> p i d", p=128)
            O("sp", "dma_start", writes=[bf("xs", 4 * b + i) for i in range(4)], dma=xl_sem[b],
              out=xs[:, 4 * b:4 * b + 4, :], in_=src)
        O("act", "activation", reads=[bf("pv")], writes=[bf("ident")], out=ident[:], in_=pv[:, PV_ID:PV_ID + 128], func=AF.Copy)
        if 1 in layers:
            O("act", "activation", reads=[bf("pv")], writes=[bf("ident32")], out=ident32[:], in_=pv[:, PV_ID:PV_ID + 128], func=AF.Copy)
            O("sp", "dma_start", writes=[bf("wr_sb")], dma=wr_sem, out=wr_sb[:], in_=wr_d[0].rearrange("(k p) n -> p k n", p=128))
        if sparse:
            O("act", "activation", reads=[bf("pv")], writes=[bf("Ubf")], out=Ubf[:], in_=pv[:, PV_U:PV_U + 128], func=AF.Copy)
            O("dve", "memset", writes=[bf("onesbf")], ap=onesbf[:], constant=1.0)
            for c in range(2):
                O("dve", "tensor_copy", reads=[bf("pv")], writes=[bf("tokid")], out=tokid[:, :, c], in_=pv[:, PV_TOK:PV_TOK + 16])

        def build_wbd(l):
            O("dve", "memset", writes=[bf("wbd")], ap=wbd[:], constant=0.0)
            for g, wd_ in enumerate((wa_d, wx_d)):
                for q in range(2):
                    src = wd_[l].rearrange("(c q) i j -> q i c j", q=2)[q]
                    dst = wbd[q * 64:(q + 1) * 64, g * 4:g * 4 + 4, q * 64:(q + 1) * 64]
                    O("pool", "dma_start", writes=[bf("wbd")], dma=wbd_sem, out=dst, in_=src)

        for l in layers:
            lam = pv[:, l * PV_L + PV_LAM:l * PV_L + PV_LAM + 4]
            c1 = cc[:, l * 4:l * 4 + 4]
            c2 = cc[:, 8 + l * 4:8 + l * 4 + 4]
            O("act", "activation", reads=[bf("pv")], writes=[bf("cc")], out=c1, in_=lam, func=AF.Exp, scale=-1.0)
            O("act", "activation", reads=[bf("cc")], writes=[bf("cc")], out=c1, in_=c1, func=AF.Ln, bias=1.0)
            O("dve", "tensor_scalar", reads=[bf("cc")], writes=[bf("cc")], out=c2, in0=c1, scalar1=-16.0, scalar2=None, op0=ALU.mult)
            O("dve", "tensor_scalar", reads=[bf("cc")], writes=[bf("cc")], out=c1, in0=c1, scalar1=-8.0, scalar2=None, op0=ALU.mult)
            O("dve", "tensor_scalar", reads=[bf("pv")], writes=[bf("hbias")], out=hbias[:, l * 8:l * 8 + 8],
              in0=pv[:, l * PV_L + PV_BA:l * PV_L + PV_BA + 8], scalar1=-1.0, scalar2=None, op0=ALU.mult)

        def pvc(l, base, off=0, n=1):
            c0 = l * PV_L + base + off
            return pv[:, c0:c0 + n]

        def rstd_block(tis):
            t0, t1 = tis[0], tis[-1] + 1
            for ti in tis:
                O("act", "activation", reads=[bf("xs", ti)], writes=[bf("junk"), bf("ss", ti)], out=s16[0][:], in_=xs[:, ti, :],
                  func=AF.Square, accum_out=ss[:, ti:ti + 1])
            sk = [bf("ss", ti) for ti in tis]
            rk = [bf("rs", ti) for ti in tis]
            O("dve", "tensor_scalar", reads=sk, writes=rk, out=rs[:, t0:t1], in0=ss[:, t0:t1], scalar1=1.0 / D, scalar2=EPS,
              op0=ALU.mult, op1=ALU.add)
            O("act", "activation", reads=rk, writes=rk, out=rs[:, t0:t1], in_=rs[:, t0:t1], func=AF.Ln)
            O("act", "activation", reads=rk, writes=rk, out=rs[:, t0:t1], in_=rs[:, t0:t1], func=AF.Exp, scale=-0.5)

        def rstd(ti):
            rstd_block([ti])

        def rmsnorm_T(ti, gcol, dst, dst_c0, dst_buf, have_rstd=False, pb_i=0):
            xb = bf("xs", ti)
            if not have_rstd:
                rstd(ti)
            xn = s16[1 + (ti % 2)]
            xnb = bf("xn", ti % 2)
            O("act", "activation", reads=[xb, bf("rs", ti)], writes=[xnb], out=xn[:], in_=xs[:, ti, :], func=AF.Copy,
              scale=rs[:, ti:ti + 1])
            pb = bf("pst", pb_i)
            G("pe", [("transpose", dict(out=pst[pb_i][:, c * 128:(c + 1) * 128], in_=xn[:, c * 128:(c + 1) * 128], identity=ident[:]))
                     for c in range(8)], reads=[xnb, bf("ident")], writes=[pb])
            gb = gcol.unsqueeze(2).broadcast_to([128, 8, 128])
            src3 = pst[pb_i][:].rearrange("p (c t) -> p c t", c=8)
            O("dve", "tensor_tensor", reads=[pb, bf("pv")], writes=[dst_buf], out=dst[:, :, dst_c0:dst_c0 + 128], in0=src3, in1=gb,
              op=ALU.mult)

        def mm_calls(out_ap, pairs):
            n = len(pairs)
            return [("matmul", dict(out=out_ap, lhsT=lhsT, rhs=rhs, start=(i == 0), stop=(i == n - 1)))
                    for i, (lhsT, rhs) in enumerate(pairs)]

        def router(ti, l):
            if sparse:
                rxn32 = hT[:, 0, :].bitcast(F32)
                rxnT32 = hT[:, 1, :].bitcast(F32).rearrange("p (c t) -> p c t", t=128)
                RXK, RTK = [bf("hTscr", 0)], [bf("hTscr", 1), bf("hTscr", 2)]
                RX0 = RXK + [bf("hT", bb) for bb in range(NB)]
            else:
                rxn32, rxnT32 = xn32, xnT32
                RXK, RTK = [bf("s32", 0), bf("s32", 1)], [bf("s32", 2), bf("s32", 3)]
                RX0 = RXK
            xb = bf("xs", ti)
            O("act", "activation", reads=[xb, bf("rs", ti)], writes=RX0, out=rxn32[:], in_=xs[:, ti, :], func=AF.Copy,
              scale=rs[:, ti:ti + 1])
            yield
            rps = pst[0][:, 0:1024].bitcast(F32)
            RPK = bf("pst", 0)
            for hh in range(2):
                G("pe", [("transpose", dict(out=rps[:, c * 128:(c + 1) * 128],
                                            in_=rxn32[:, (4 * hh + c) * 128:(4 * hh + c + 1) * 128], identity=ident32[:]))
                         for c in range(4)], reads=RXK + [bf("ident32")], writes=[RPK])
                gb = pvc(l, PV_GFFN, 4 * hh, 4).unsqueeze(2).broadcast_to([128, 4, 128])
                O("dve", "tensor_tensor", reads=[RPK, bf("pv")], writes=[RTK[hh]],
                  out=rxnT32[:, 4 * hh:4 * hh + 4, :], in0=rps.rearrange("p (c t) -> p c t", c=4), in1=gb, op=ALU.mult)
                yield
            G("pe", mm_calls(rps[:, 0:NE], [(rxnT32[:, c, :], wr_sb[:, c, :]) for c in range(8)]),
              reads=RTK + [bf("wr_sb")], writes=[RPK])
            lgt = lg[:]
            cbt = comb[:, ti, :]
            O("act", "activation", reads=[RPK], writes=[bf("lg")], out=lgt, in_=rps[:, 0:NE], func=AF.Copy)
            yield
            O("dve", "scalar_tensor_tensor", reads=[bf("lg"), bf("pv")], writes=[bf("lg")], out=lgt, in0=pv[:, PV_IOTA:PV_IOTA + NE],
              scalar=-1e-30, in1=lgt, op0=ALU.mult, op1=ALU.add)
            O("dve", "max", reads=[bf("lg")], writes=[bf("m8")], out=m8[:], in_=lgt)
            O("dve", "tensor_scalar", reads=[bf("lg"), bf("m8")], writes=[bf("comb", ti)], out=cbt, in0=lgt, scalar1=m8[:, 1:2],
              scalar2=None, op0=ALU.is_ge)
            if sparse:
                O("dve", "tensor_copy", reads=[bf("comb", ti)], writes=[bf("msk", ti)], out=msk[:, ti, :], in_=cbt)
                O("dve", "tensor_scalar", reads=[bf("lg"), bf("m8")], writes=[bf("m1", ti)], out=m1[:, ti, :], in0=lgt,
                  scalar1=m8[:, 0:1], scalar2=None, op0=ALU.is_equal)
            O("dve", "tensor_scalar", reads=[bf("m8")], writes=[bf("rsum")], out=rsum[:, 0:1], in0=m8[:, 0:1], scalar1=-1.0,
              scalar2=None, op0=ALU.mult)
            O("act", "activation", reads=[bf("lg"), bf("rsum")], writes=[bf("ex")], out=ex[:], in_=lgt, func=AF.Exp,
              bias=rsum[:, 0:1], scale=1.0)
            O("dve", "tensor_tensor", reads=[bf("comb", ti), bf("ex")], writes=[bf("comb", ti)], out=cbt, in0=cbt, in1=ex[:], op=ALU.mult)
            O("dve", "reduce_sum", reads=[bf("comb", ti)], writes=[bf("rsum")], out=rsum[:, 1:2], in_=cbt, axis=mybir.AxisListType.X)
            O("dve", "reciprocal", reads=[bf("rsum")], writes=[bf("rsum")], out=rsum[:, 1:2], in_=rsum[:, 1:2])
            O("dve", "tensor_scalar", reads=[bf("comb", ti), bf("rsum")], writes=[bf("comb", ti)], out=cbt, in0=cbt,
              scalar1=rsum[:, 1:2], scalar2=None, op0=ALU.mult)
            yield

        def ffn(l):
            experts = [None] if l == 0 else list(range(n_experts))
            items = [(e_, g, ti) for e_ in experts for g in range(ffn_groups) for ti in range(NT)]
            slots = {}

            def stage_gu(n):
                e_, g, ti = items[n]
                if ti == 0:
                    if e_ is None:
                        sg_, su_, sd_ = fg_d[0], fu_d[0], fd_d[0]
                    else:
                        sg_, su_, sd_ = mg_d[0][e_], mu_d[0][e_], md_d[0][e_]
                    a = load_w(sg_[:, g * 512:(g + 1) * 512].rearrange("(k p) n -> p k n", p=128))
                    b_ = load_w(su_[:, g * 512:(g + 1) * 512].rearrange("(k p) n -> p k n", p=128))
                    c_ = load_w(sd_[g * 512:(g + 1) * 512, :].rearrange("(c p) (h n) -> p c h n", p=128, h=2), split=True)
                    slots[(e_, g)] = (a, b_, c_)
                a, b_, c_ = slots[(e_, g)]
                pg, pu = n % 2, 2 + n % 2
                hb = bf("hT", ti // 4)
                calls = []
                for k in range(8):
                    lhsT = hT[:, k, ti * 128:(ti + 1) * 128]
                    calls.append(("matmul", dict(out=psf[pg][:], lhsT=lhsT, rhs=ring[a][:, k, :], start=(k == 0), stop=(k == 7))))
                    calls.append(("matmul", dict(out=psf[pu][:], lhsT=lhsT, rhs=ring[b_][:, k, :], start=(k == 0), stop=(k == 7))))
                G("pe", calls, reads=[hb, bf("ring", a), bf("ring", b_)], writes=[bf("psf", pg), bf("psf", pu)])
                sil = s32[n % 2]
                O("act", "activation", reads=[bf("psf", pg)], writes=[bf("s32", n % 2)], out=sil[:], in_=psf[pg][:], func=AF.Silu)
                O("dve", "tensor_tensor", reads=[bf("s32", n % 2), bf("psf", pu)], writes=[bf(*HIDK[n % 2])], out=hid[n % 2][:, 0:512],
                  in0=sil[:], in1=psf[pu][:], op=ALU.mult)

            def stage_tr(n):
                pt = n % 2
                G("pe", [("transpose", dict(out=pst[pt][:, c * 128:(c + 1) * 128], in_=hid[n % 2][:, c * 128:(c + 1) * 128],
                                            identity=ident[:])) for c in range(4)],
                  reads=[bf(*HIDK[n % 2]), bf("ident")], writes=[bf("pst", pt)])
                O("act", "activation", reads=[bf("pst", pt)], writes=[bf(*k) for k in HIDTK[n % 2]], out=hidT[n % 2][:, 0:512], in_=pst[pt][:, 0:512],
                  func=AF.Copy)

            def stage_dn(n):
                e_, g, ti = items[n]
                c_ = slots[(e_, g)][2]
                xb = bf("xs", ti)
                for h in range(2):
                    bank = 4 + h
                    pairs = [(hidT[n % 2][:, c * 128:(c + 1) * 128], ring[c_][:, 2 * c + h, :]) for c in range(4)]
                    G("pe", mm_calls(psf[bank][:], pairs), reads=[bf(*k) for k in HIDTK[n % 2]] + [bf("ring", c_)], writes=[bf("psf", bank)])
                    xo = xs[:, ti, h * 512:(h + 1) * 512]
                    if e_ is None:
                        O("dve", "tensor_tensor", reads=[bf("psf", bank), xb], writes=[xb], out=xo, in0=psf[bank][:], in1=xo, op=ALU.add)
                    else:
                        O("dve", "scalar_tensor_tensor", reads=[bf("psf", bank), xb, bf("comb", ti)], writes=[xb], out=xo,
                          in0=psf[bank][:], scalar=comb[:, ti, e_:e_ + 1], in1=xo, op0=ALU.mult, op1=ALU.add)

            N = len(items)
            for s in range(N + 2):
                if s < N:
                    stage_gu(s)
                if 0 <= s - 1 < N:
                    stage_tr(s - 1)
                if 0 <= s - 2 < N:
                    stage_dn(s - 2)

        def moe_sparse(l):
            IOA = bass.IndirectOffsetOnAxis
            slots = {}
            for g in range(2):
                a = load_w(mg_d[0][0][:, g * 512:(g + 1) * 512].rearrange("(k p) n -> p k n", p=128))
                b_ = load_w(mu_d[0][0][:, g * 512:(g + 1) * 512].rearrange("(k p) n -> p k n", p=128))
                c_ = load_w(md_d[0][0][g * 512:(g + 1) * 512, :].rearrange("(c p) (h n) -> p c h n", p=128, h=2), split=True)
                slots[(0, g, "m")] = (a, b_, c_)
            P.barrier()
            f2 = lambda t: t[:].rearrange("p i e -> p (i e)")
            v3 = lambda a: a.rearrange("p (i e) -> p i e", e=NE)
            mskf, m1f, combf = f2(msk), f2(m1), f2(comb)
            K2, K3, K4, K5, K6 = (bf("s32", i) for i in (2, 3, 4, 5, 1))
            tot, off, pos, tmp, m2f = (s32[i][:, 0:128] for i in (2, 3, 4, 5, 1))
            allm = [bf("msk", i) for i in range(NT)] + [bf("m1", i) for i in range(NT)] + [bf("comb", i) for i in range(NT)]
            O("act", "activation", reads=allm, writes=[bf("mskb")], out=mskb[:], in_=mskf, func=AF.Copy)
            G("pe", [("matmul", dict(out=psf[0][:, 0:128], lhsT=Ubf[:], rhs=mskb[:], start=True, stop=True))],
              reads=[bf("Ubf"), bf("mskb")], writes=[bf("psf", 0)])
            G("pe", [("matmul", dict(out=psf[1][:, 0:128], lhsT=onesbf[:], rhs=mskb[:], start=True, stop=True))],
              reads=[bf("onesbf"), bf("mskb")], writes=[bf("psf", 1)])
            O("dve", "tensor_copy", reads=[bf("psf", 1)], writes=[K2], out=tot, in_=psf[1][:, 0:128])
            O("dve", "memset", writes=[K3], ap=off[:, 0:NE], constant=0.0)
            for i in range(1, NT):
                O("dve", "tensor_tensor", reads=[K2, K3], writes=[K3], out=off[:, NE * i:NE * i + NE], in0=off[:, NE * (i - 1):NE * i],
                  in1=tot[:, NE * (i - 1):NE * i], op=ALU.add)
            O("dve", "tensor_tensor", reads=[bf("psf", 0), K3], writes=[K4], out=pos, in0=psf[0][:, 0:128], in1=off, op=ALU.add)
            O("dve", "tensor_tensor", reads=[K2, K3], writes=[bf("nef")], out=nef[:], in0=off[:, 120:128], in1=tot[:, 120:128], op=ALU.add)
            O("dve", "tensor_copy", reads=[bf("nef")], writes=[bf("cnt")], out=cnt_i[:], in_=nef[:])
            O("dve", "tensor_tensor", reads=allm, writes=[K6], out=m2f, in0=mskf, in1=m1f, op=ALU.subtract)
            iota3 = pv[:, PV_IOTA:PV_IOTA + NE].unsqueeze(1).broadcast_to([128, NT, NE])
            for c, mc in ((0, m1f), (1, m2f)):
                for q, other in ((0, pos), (1, None), (2, combf)):
                    if other is None:
                        O("dve", "tensor_tensor", reads=allm + [K6, bf("pv")], writes=[K5], out=v3(tmp), in0=v3(mc), in1=iota3, op=ALU.mult)
                    else:
                        O("dve", "tensor_tensor", reads=allm + [K6, K4], writes=[K5], out=tmp, in0=mc, in1=other, op=ALU.mult)
                    O("dve", "reduce_sum", reads=[K5], writes=[bf("PEG")], out=PEG[:, q, 16 * c:16 * c + 16], in_=v3(tmp),
                      axis=mybir.AxisListType.X)
            Pc, Ec, Gc = PEG[:, 0, :], PEG[:, 1, :], PEG[:, 2, :]
            O("dve", "scalar_tensor_tensor", reads=[bf("PEG")], writes=[K5], out=tmp[:, 0:32], in0=Ec, scalar=float(S), in1=Pc,
              op0=ALU.mult, op1=ALU.add)
            O("dve", "tensor_scalar", reads=[K5], writes=[K5], out=tmp[:, 0:32], in0=tmp[:, 0:32], scalar1=float(NE * S - 1), scalar2=0.0,
              op0=ALU.min, op1=ALU.max)
            O("dve", "tensor_copy", reads=[K5], writes=[bf("Ysel")], out=Ysel[:], in_=tmp[:, 0:32])
            Pi, jj = ti32[:, 0, :], ti32[:, 1, :]
            jjf, t0 = tmp[:, 32:64], tmp[:, 64:96]
            O("dve", "tensor_copy", reads=[bf("PEG")], writes=[bf("ti32")], out=Pi, in_=Pc)
            O("dve", "tensor_scalar", reads=[bf("ti32")], writes=[bf("ti32")], out=jj, in0=Pi, scalar1=7, scalar2=None, op0=ALU.arith_shift_right)
            O("dve", "tensor_copy", reads=[bf("ti32")], writes=[K5], out=jjf, in_=jj)
            O("dve", "tensor_scalar", reads=[bf("PEG")], writes=[K5], out=t0, in0=Pc, scalar1=128.0, scalar2=None, op0=ALU.mult)
            O("dve", "scalar_tensor_tensor", reads=[K5], writes=[K5], out=t0, in0=jjf, scalar=-16383.0, in1=t0, op0=ALU.mult, op1=ALU.add)
            O("dve", "scalar_tensor_tensor", reads=[K5, bf("PEG")], writes=[K5], out=t0, in0=Ec, scalar=16.0, in1=t0, op0=ALU.mult, op1=ALU.add)
            O("dve", "tensor_scalar", reads=[K5], writes=[K5], out=t0, in0=t0, scalar1=float(128 * 128 - 1), scalar2=0.0, op0=ALU.min,
              op1=ALU.max)
            O("dve", "tensor_copy", reads=[K5], writes=[bf("Tsc")], out=Tsc[:], in_=t0)
            O("dve", "memset", writes=[bf("idx_all")], ap=idx_all[:], constant=0)
            O("sp", "dma_start", reads=[bf("idx_all")], writes=[bf("T2")], dma=t2_sem, out=T2_d.rearrange("(p a) c -> p (a c)", p=128),
              in_=idx_all[:])
            for c in range(2):
                for i in range(NT):
                    O("pool", "indirect_dma_start", reads=[bf("Tsc"), bf("tokid"), bf("T2")], writes=[bf("T2s", c, i)], dma=sc_sem, out=T2_d,
                      out_offset=IOA(ap=Tsc[:, 16 * c + i:16 * c + i + 1], axis=0), in_=tokid[:, i, :], in_offset=None)
            O("sp", "dma_start", reads=[bf("T2")] + [bf("T2s", c, i) for c in range(2) for i in range(NT)], writes=[bf("idx_all")],
              dma=ix_sem, out=idx_all[:],
              in_=T2_d.rearrange("(p a) c -> p (a c)", p=128))

            gffn3 = pvc(l, PV_GFFN, 0, 8).unsqueeze(2).broadcast_to([128, 8, 128])
            Hall = [bf("H", i) for i in range(NT)]
            npro = [0]

            def guard(e_, j):
                P.guard = (e_ % 2, 128 * j + 1)

            def prologue(e_, j):
                n = npro[0]
                npro[0] += 1
                if j == 0:
                    P.guard = None
                    P.regload(e_ % 2, cnt_i[0:1, e_:e_ + 1], [bf("cnt")])
                guard(e_, j)
                kk = bf("s32", 2 + n % 4)
                hg = s32big[:, 2 + n % 4, :].bitcast(BF16)
                col = 2 * (e_ * 16 + j)
                O("pool", "indirect_dma_start", reads=Hall + [bf("idx_all")], writes=[kk], dma=g_sem[n % 4], out=hg, out_offset=None,
                  in_=H_d, in_offset=IOA(ap=idx_all[:, col:col + 1], axis=0))
                pb = bf("pst", n % 2)
                G("pe", [("transpose", dict(out=pst[n % 2][:, c * 128:(c + 1) * 128], in_=hg[:, c * 128:(c + 1) * 128], identity=ident[:]))
                         for c in range(8)], reads=[kk, bf("ident")], writes=[pb])
                O("dve", "tensor_tensor", reads=[pb, bf("pv")], writes=[bf("hTj", j)], out=hT[:, :, j * 128:(j + 1) * 128],
                  in0=pst[n % 2][:].rearrange("p (c t) -> p c t", c=8), in1=gffn3, op=ALU.mult)
                P.guard = None

            def stage_gu(items, n, j0, tag):
                e_, g, j = items[n]
                if j == j0 and (e_, g, tag) not in slots:
                    a = load_w(mg_d[0][e_][:, g * 512:(g + 1) * 512].rearrange("(k p) n -> p k n", p=128))
                    b_ = load_w(mu_d[0][e_][:, g * 512:(g + 1) * 512].rearrange("(k p) n -> p k n", p=128))
                    c_ = load_w(md_d[0][e_][g * 512:(g + 1) * 512, :].rearrange("(c p) (h n) -> p c h n", p=128, h=2), split=True)
                    slots[(e_, g, tag)] = (a, b_, c_)
                a, b_, c_ = slots[(e_, g, tag)]
                pg, pu = n % 2, 2 + n % 2
                guard(e_, j)
                calls = []
                for k in range(8):
                    lhsT = hT[:, k, j * 128:(j + 1) * 128]
                    calls.append(("matmul", dict(out=psf[pg][:], lhsT=lhsT, rhs=ring[a][:, k, :], start=(k == 0), stop=(k == 7))))
                    calls.append(("matmul", dict(out=psf[pu][:], lhsT=lhsT, rhs=ring[b_][:, k, :], start=(k == 0), stop=(k == 7))))
                G("pe", calls, reads=[bf("hTj", j), bf("ring", a), bf("ring", b_)], writes=[bf("psf", pg), bf("psf", pu)])
                sil = s32[n % 2]
                P.guard = None
                O("act", "activation", reads=[bf("psf", pg)], writes=[bf("s32", n % 2)], out=sil[:], in_=psf[pg][:], func=AF.Silu)
                guard(e_, j)
                O("dve", "tensor_tensor", reads=[bf("s32", n % 2), bf("psf", pu)], writes=[bf(*HIDK[n % 2])], out=hid[n % 2][:, 0:512],
                  in0=sil[:], in1=psf[pu][:], op=ALU.mult)
                P.guard = None
                if tag == "m" and g == ffn_groups - 1 and e_ + 1 < n_experts:
                    prologue(e_ + 1, j)

            def stage_tr(items, n, j0, tag):
                e_, g, j = items[n]
                pt = n % 2
                guard(e_, j)
                G("pe", [("transpose", dict(out=pst[pt][:, c * 128:(c + 1) * 128], in_=hid[n % 2][:, c * 128:(c + 1) * 128],
                                            identity=ident[:])) for c in range(4)],
                  reads=[bf(*HIDK[n % 2]), bf("ident")], writes=[bf("pst", pt)])
                P.guard = None
                O("act", "activation", reads=[bf("pst", pt)], writes=[bf(*k) for k in HIDTK[n % 2]], out=hidT[n % 2][:, 0:512], in_=pst[pt][:, 0:512],
                  func=AF.Copy)

            def stage_dn(items, n, j0, tag):
                e_, g, j = items[n]
                c_ = slots[(e_, g, tag)][2]
                xb = bf("xs", j)
                guard(e_, j)
                calls = []
                for h in range(2):
                    pairs = [(hidT[n % 2][:, c * 128:(c + 1) * 128], ring[c_][:, 2 * c + h, :]) for c in range(4)]
                    calls += mm_calls(psf[4 + h][:], pairs)
                G("pe", calls, reads=[bf(*k) for k in HIDTK[n % 2]] + [bf("ring", c_)], writes=[bf("psf", 4), bf("psf", 5)])
                for h in range(2):
                    bank = 4 + h
                    xo = xs[:, j, h * 512:(h + 1) * 512]
                    if g == 0:
                        O("dve", "tensor_copy", reads=[bf("psf", bank)], writes=[xb], out=xo, in_=psf[bank][:])
                    else:
                        O("dve", "tensor_tensor", reads=[bf("psf", bank), xb], writes=[xb], out=xo, in0=psf[bank][:], in1=xo, op=ALU.add)
                if g == ffn_groups - 1:
                    r0 = e_ * S + j * 128
                    O("sp", "dma_start", reads=[xb], writes=[bf("Y")], dma=y_sem, out=Y_d[r0:r0 + 128, :], in_=xs[:, j, :])
                P.guard = None

            def run_items(items, j0, tag):
                N = len(items)
                for s_ in range(N + 2):
                    if s_ < N:
                        stage_gu(items, s_, j0, tag)
                    if 0 <= s_ - 1 < N:
                        stage_tr(items, s_ - 1, j0, tag)
                    if 0 <= s_ - 2 < N:
                        stage_dn(items, s_ - 2, j0, tag)

            for j in range(JMAX):
                prologue(0, j)
            run_items([(e_, g, j) for e_ in range(n_experts) for g in range(ffn_groups) for j in range(JMAX)], 0, "m")
            for e_ in range(n_experts):
                P.regload(e_ % 2, cnt_i[0:1, e_:e_ + 1], [bf("cnt")])
                P.region_begin(e_ % 2, 128 * JMAX + 1)
                for j in range(JMAX, NT):
                    prologue(e_, j)
                run_items([(e_, g, j) for g in range(ffn_groups) for j in range(JMAX, NT)], JMAX, "o")
                P.region_end()

            for i in range(NT):
                xb = bf("xs", i)
                O("sp", "dma_start", reads=[bf("Xs", t) for t in range(NT)], writes=[xb], dma=xr_sem[i], out=xs[:, i, :], in_=Xs_d[i * 128:(i + 1) * 128, :])
                for c in range(2):
                    n = 2 * i + c
                    q = n % 6
                    keys = [bf("s32", 2 * q), bf("s32", 2 * q + 1)]
                    yg = s32big[:, 2 * q:2 * q + 2, :].rearrange("p a n -> p (a n)")
                    O("pool", "indirect_dma_start", reads=[bf("Y"), bf("Ysel")], writes=keys, dma=yg_sem[q], out=yg, out_offset=None,
                      in_=Y_d, in_offset=IOA(ap=Ysel[:, 16 * c + i:16 * c + i + 1], axis=0))
                    O("dve", "scalar_tensor_tensor", reads=keys + [xb, bf("PEG")], writes=[xb], out=xs[:, i, :], in0=yg,
                      scalar=PEG[:, 2, 16 * c + i:16 * c + i + 1], in1=xs[:, i, :], op0=ALU.mult, op1=ALU.add)

        for l in layers:
            build_wbd(l)
            O("dve", "memset", writes=[bf("ctail", j) for j in range(4)], ap=ctail[:], constant=0.0)
            O("dve", "memset", writes=[bf("utail", j) for j in range(4)], ap=utail[:], constant=0.0)
            for j in range(4):
                O("dve", "memset", writes=[bf("hlast", j)], ap=hlast[:, j:j + 1], constant=0.0)
            def win_tile(t):
                return load_w(w_in_d[l][:, t * 512:(t + 1) * 512].rearrange("(k p) n -> p k n", p=128))

            def proj(slot_i, oc, bank):
                pairs = [(ring[slot_i][:, k, (oc % 4) * 128:(oc % 4) * 128 + 128], hTm[:, k, :]) for k in range(8)]
                G("pe", mm_calls(psf[bank][:], pairs), reads=[bf("ring", slot_i), bf("hTm")], writes=[bf("psf", bank)])

            def gen_A(b):
                yT_b = bf("yT")
                sc, sv, sbg = win_tile(0), win_tile(2), win_tile(1)
                yield
                for j in range(4):
                    k0, k1 = 0, 1
                    csb, acc = s32[k0], s32[k1]
                    K0, K1 = bf("s32", k0), bf("s32", k1)
                    cvb = bf("cvs")
                    proj(sc, j, 0)
                    yield
                    O("act", "activation", reads=[bf("psf", 0)], writes=[K0], out=csb[:], in_=psf[0][:], func=AF.Copy)
                    proj(sv, 8 + j, 1)
                    yield
                    O("act", "activation", reads=[bf("ctail", j)], writes=[cvb], out=cvs[:, 0:2], in_=ctail[:, j, :], func=AF.Copy)
                    O("dve", "tensor_tensor", reads=[K0, bf("psf", 1)], writes=[cvb], out=cvs[:, 2:514], in0=csb[:],
                      in1=psf[1][:], op=ALU.mult)
                    yield
                    O("dve", "tensor_scalar", reads=[cvb, bf("pv")], writes=[K1], out=acc[:], in0=cvs[:, 0:512],
                      scalar1=pvc(l, PV_CONV, j), scalar2=None, op0=ALU.mult)
                    yield
                    for k in (1, 2):
                        O("dve", "scalar_tensor_tensor", reads=[cvb, K1, bf("pv")], writes=[K1], out=acc[:],
                          in0=cvs[:, k:k + 512], scalar=pvc(l, PV_CONV, k * 4 + j), in1=acc[:], op0=ALU.mult, op1=ALU.add)
                        yield
                    O("act", "activation", reads=[cvb], writes=[bf("ctail", j)], out=ctail[:, j, :], in_=cvs[:, 512:514], func=AF.Copy)
                    proj(sbg, 4 + j, 0)
                    O("dve", "tensor_tensor", reads=[K1, bf("psf", 0)], writes=[yT_b], out=yT[:, j, :], in0=acc[:],
                      in1=psf[0][:], op=ALU.mult)
                    yield

            def gen_B(b, sid, su):
                yT_b = bf("yT")
                ku, kr, ki, ka = (2, 3, 4, 5) if sid == 0 else (8, 9, 10, 11)
                ups = ups_[sid]
                bu, bi = (3, 4) if sid == 0 else (5, 2)
                pu_, pr, pi_ = psf[bu][:], psf[bu][:], psf[bi][:]
                PUK, PRK, PIK = bf("psf", bu), bf("psf", bu), bf("psf", bi)
                KU, KR, KI, KA = bf("s32", ku), bf("s32", kr), bf("s32", ki), bf("s32", ka)
                yield
                for j in (sid, sid + 2):
                    uc, r_, i_, a_ = s32[ku], s32[kr], s32[ki], s32[ka]
                    ucb = s16[3][:, sid * 512:(sid + 1) * 512]
                    UCB = bf("ucb", sid)
                    ub = bf("ups", sid)
                    proj(su, 12 + j, bu)
                    yield
                    O("act", "activation", reads=[bf("utail", j)], writes=[ub], out=ups[:, 0:3], in_=utail[:, j, :], func=AF.Copy)
                    O("act", "activation", reads=[PUK], writes=[ub], out=ups[:, 3:515], in_=pu_, func=AF.Copy)
                    yield
                    O("dve", "tensor_scalar", reads=[ub, bf("pv")], writes=[KU], out=uc[:], in0=ups[:, 0:512],
                      scalar1=pvc(l, PV_LCW, j), scalar2=pvc(l, PV_LCB, j), op0=ALU.mult, op1=ALU.add)
                    yield
                    for k in (1, 2, 3):
                        O("dve", "scalar_tensor_tensor", reads=[ub, KU, bf("pv")], writes=[KU], out=uc[:],
                          in0=ups[:, k:k + 512], scalar=pvc(l, PV_LCW, k * 4 + j), in1=uc[:], op0=ALU.mult, op1=ALU.add)
                        yield
                    O("act", "activation", reads=[ub], writes=[bf("utail", j)], out=utail[:, j, :], in_=ups[:, 512:515], func=AF.Copy)
                    O("act", "activation", reads=[KU], writes=[UCB], out=ucb, in_=uc[:], func=AF.Copy)
                    yield
                    G("pe", [("matmul", dict(out=pr, lhsT=wbd[:, j, :], rhs=ucb, start=True, stop=True))],
                      reads=[bf("wbd"), UCB], writes=[PRK])
                    G("pe", [("matmul", dict(out=pi_, lhsT=wbd[:, 4 + j, :], rhs=ucb, start=True, stop=True))],
                      reads=[bf("wbd"), UCB], writes=[PIK])
                    yield
                    for (t_, ps_, PK_, K_, hb_) in ((r_, pr, PRK, KR, l * 8 + j), (i_, pi_, PIK, KI, l * 8 + 4 + j)):
                        O("act", "activation", reads=[PK_, bf("hbias")], writes=[K_], out=t_[:], in_=ps_,
                          func=AF.Exp, bias=hbias[:, hb_:hb_ + 1], scale=-1.0)
                        yield
                        O("act", "activation", reads=[K_], writes=[K_], out=t_[:], in_=t_[:], func=AF.Ln, bias=1.0)
                        yield
                        O("act", "activation", reads=[K_], writes=[K_], out=t_[:], in_=t_[:], func=AF.Exp, scale=-1.0)
                        yield
                    O("dve", "tensor_tensor", reads=[KI, KU], writes=[KI], out=i_[:], in0=i_[:], in1=uc[:],
                      op=ALU.mult)
                    O("act", "activation", reads=[KR, bf("cc")], writes=[KA], out=a_[:], in_=r_[:], func=AF.Exp,
                      scale=cc[:, l * 4 + j:l * 4 + j + 1])
                    yield
                    O("act", "activation", reads=[KR, bf("cc")], writes=[KR], out=r_[:], in_=r_[:], func=AF.Exp,
                      scale=cc[:, 8 + l * 4 + j:8 + l * 4 + j + 1])
                    yield
                    O("act", "activation", reads=[KR], writes=[KR], out=r_[:], in_=r_[:], func=AF.Ln, bias=1.0000001, scale=-1.0)
                    yield
                    O("act", "activation", reads=[KR], writes=[KR], out=r_[:], in_=r_[:], func=AF.Exp, scale=0.5)
                    yield
                    O("dve", "tensor_tensor", reads=[KI, KR], writes=[KI], out=i_[:], in0=i_[:], in1=r_[:],
                      op=ALU.mult)
                    yield
                    hlb = bf("hlast", j)
                    O("dve", "tensor_tensor_scan", reads=[KA, KI, hlb], writes=[KU], out=uc[:],
                      data0=a_[:], data1=i_[:], initial=hlast[:, j:j + 1], op0=ALU.mult, op1=ALU.add)
                    yield
                    O("act", "activation", reads=[KU], writes=[hlb], out=hlast[:, j:j + 1], in_=uc[:, 511:512], func=AF.Copy)
                    yield ("wait", ("gelu", b, j))
                    O("dve", "tensor_tensor", reads=[bf("s32", 6 + j % 2), KU], writes=[yT_b], out=yT[:, 4 + j, :],
                      in0=s32[6 + j % 2][:], in1=uc[:], op=ALU.mult)
                    yield ("signal", ("yb", b, j))

            def gen_C(b):
                sg = win_tile(4)
                gps = pst[1][:, 0:1024].bitcast(F32)
                yield
                for j in range(4):
                    qk = 6 + j % 2
                    q_, QK = s32[qk], bf("s32", qk)
                    if j >= 2:
                        yield ("wait", ("yb", b, j - 2))
                    pairs = [(ring[sg][:, k, j * 128:j * 128 + 128], hTm[:, k, :]) for k in range(8)]
                    G("pe", mm_calls(gps, pairs), reads=[bf("ring", sg), bf("hTm")], writes=[bf("pst", 1)])
                    yield
                    O("act", "activation", reads=[bf("pst", 1)], writes=[QK], out=q_[:], in_=gps, func=AF.Square)
                    yield
                    O("dve", "tensor_scalar", reads=[QK], writes=[QK], out=q_[:], in0=q_[:], scalar1=GC1, scalar2=1.0,
                      op0=ALU.mult, op1=ALU.add)
                    yield
                    O("dve", "tensor_tensor", reads=[QK, bf("pst", 1)], writes=[QK], out=q_[:], in0=q_[:], in1=gps, op=ALU.mult)
                    yield
                    O("act", "activation", reads=[QK], writes=[QK], out=q_[:], in_=q_[:], func=AF.Exp, scale=-2.0 * GC0)
                    yield
                    O("act", "activation", reads=[QK], writes=[QK], out=q_[:], in_=q_[:], func=AF.Ln, bias=1.0)
                    yield
                    O("act", "activation", reads=[QK], writes=[QK], out=q_[:], in_=q_[:], func=AF.Exp, scale=-1.0)
                    yield
                    O("dve", "tensor_tensor", reads=[QK, bf("pst", 1)], writes=[QK], out=q_[:], in0=q_[:], in1=gps, op=ALU.mult)
                    yield ("signal", ("gelu", b, j))

            def tail_wout(b):
                yT_b = bf("yT")
                so = [load_w(w_out_d[l][:, h * 512:(h + 1) * 512].rearrange("(k p) n -> p k n", p=128)) for h in range(2)]
                for i in range(4):
                    ti = 4 * b + i
                    xb = bf("xs", ti)
                    for h in range(2):
                        bank = 1 + h
                        pairs = [(yT[:, k, i * 128:(i + 1) * 128], ring[so[h]][:, k, :]) for k in range(8)]
                        G("pe", mm_calls(psf[bank][:], pairs), reads=[yT_b, bf("ring", so[h])], writes=[bf("psf", bank)])
                        xo = xs[:, ti, h * 512:(h + 1) * 512]
                        O("dve", "tensor_tensor", reads=[bf("psf", bank), xb], writes=[xb], out=xo, in0=psf[bank][:], in1=xo, op=ALU.add)

            def gen_tail(b):
                yield
                rstd_block([4 * b + i for i in range(4)])
                yield
                for i in range(4):
                    ti = 4 * b + i
                    if l == 1 and sparse:
                        xb = bf("xs", ti)
                        xn = s16[1 + (ti % 2)]
                        xnb = bf("xn", ti % 2)
                        O("act", "activation", reads=[xb, bf("rs", ti)], writes=[xnb], out=xn[:], in_=xs[:, ti, :], func=AF.Copy,
                          scale=rs[:, ti:ti + 1])
                        O("sp", "dma_start", reads=[xnb], writes=[bf("H", ti)], dma=h_sem[ti % 2], out=H_d[ti * 128:(ti + 1) * 128, :], in_=xn[:])
                        yield
                        yield from router(ti, l)
                        O("sp", "dma_start", reads=[xb], writes=[bf("Xs", ti)], dma=xsp_sem, out=Xs_d[ti * 128:(ti + 1) * 128, :],
                          in_=xs[:, ti, :])
                        yield
                    else:
                        rmsnorm_T(ti, pvc(l, PV_GFFN, 0, 8), hT, ti * 128, bf("hT", b), have_rstd=True)
                        yield
                        if l == 1:
                            yield from router(ti, l)

            def run_streams(gens):
                gens = list(gens)
                blocked, done = {}, set()
                while gens:
                    progressed = False
                    for g_ in list(gens):
                        if id(g_) in blocked:
                            if blocked[id(g_)] not in done:
                                continue
                            del blocked[id(g_)]
                        progressed = True
                        try:
                            r = next(g_)
                        except StopIteration:
                            gens.remove(g_)
                            continue
                        if r is not None:
                            if r[0] == "signal":
                                done.add(r[1])
                            elif r[1] not in done:
                                blocked[id(g_)] = r[1]
                    assert progressed, "stream deadlock"

            for b in range(NB):
                rstd_block([4 * b + i for i in range(4)])
                for i in range(4):
                    rmsnorm_T(4 * b + i, pvc(l, PV_GMIX, 0, 8), hTm, i * 128, bf("hTm"), have_rstd=True, pb_i=i % 2)
                su_ = win_tile(3)
                run_streams([gen_B(b, 0, su_), gen_B(b, 1, su_), gen_C(b), gen_A(b)] + ([gen_tail(b - 1)] if b > 0 else []))
                tail_wout(b)
            run_streams([gen_tail(NB - 1)])
            if l == 1 and sparse:
                moe_sparse(l)
            else:
                ffn(l)

        otoks = []
        if final_norm:
            gfin = xn32
            O("sp", "dma_start", writes=[bf("s32", 0), bf("s32", 1)], dma=gf_sem, out=xn32[:], in_=gfin_d)
        if final_norm:
            rstd_block(list(range(NT)))
        for ti in range(NT):
            xb = bf("xs", ti)
            if final_norm:
                O("dve", "scalar_tensor_tensor", reads=[xb, bf("rs", ti), bf("s32", 0), bf("s32", 1)], writes=[xb], out=xs[:, ti, :], in0=xs[:, ti, :],
                  scalar=rs[:, ti:ti + 1], in1=gfin[:], op0=ALU.mult, op1=ALU.mult)
            otoks.append(O("sp", "dma_start", reads=[xb], dma=out_sem, out=out_d[ti * 128:(ti + 1) * 128, :], in_=xs[:, ti, :]))
        P.wait("sp", otoks)
        P.emit()
    return nc


def pack_pv(inp):
    pv = np.zeros((128, PV_N), np.float32)

    def col(v):
        return np.asarray(v, np.float32).reshape(-1, 128).T

    for l in range(2):
        o = l * PV_L
        pv[:, o + PV_GMIX:o + PV_GMIX + 8] = col(inp["norm_mix"][l])
        pv[:, o + PV_GFFN:o + PV_GFFN + 8] = col(inp["norm_ffn"][l])
        pv[:, o + PV_CONV:o + PV_CONV + 12] = np.asarray(inp["conv_w"][l], np.float32).reshape(3, 4, 128).transpose(2, 0, 1).reshape(128, 12)
        pv[:, o + PV_LCW:o + PV_LCW + 16] = np.asarray(inp["lru_conv_w"][l], np.float32).reshape(4, 4, 128).transpose(2, 0, 1).reshape(128, 16)
        pv[:, o + PV_LCB:o + PV_LCB + 4] = col(inp["lru_conv_b"][l])
        pv[:, o + PV_BA:o + PV_BA + 4] = col(inp["lru_ba"][l])
        pv[:, o + PV_BX:o + PV_BX + 4] = col(inp["lru_bx"][l])
        pv[:, o + PV_LAM:o + PV_LAM + 4] = col(inp["lru_lambda"][l])
    pv[:, PV_GFIN:PV_GFIN + 8] = col(inp["norm_final"])
    pv[:, PV_ID:PV_ID + 128] = np.eye(128, dtype=np.float32)
    pv[:, PV_U:PV_U + 128] = np.triu(np.ones((128, 128), np.float32), k=1)
    pv[:, PV_TOK:PV_TOK + 16] = (128.0 * np.arange(16)[None, :] + np.arange(128)[:, None]).astype(np.float32)
    pv[:, PV_IOTA:PV_IOTA + 8] = np.arange(8, dtype=np.float32)[None, :]
    return pv


_W0 = ("ffn_w_gate", "ffn_w_up", "ffn_w_down")
_W1 = ("w_router", "moe_w_gate", "moe_w_up", "moe_w_down")
_WC = ("w_in", "w_out", "lru_wa", "lru_wx")
FUSED = True


def _launch(nc, x, inp, names, pv, gfin):
    n = x.shape[0]
    shared = {k: np.ascontiguousarray(np.asarray(inp[k], np.float32)) for k in names}
    in_maps = []
    for c in range(n):
        m = {"x": np.ascontiguousarray(x[c]), "pv": pv, "gfin": gfin}
        m.update(shared)
        in_maps.append(m)
    res = run_bass_kernel_spmd(nc, in_maps, core_ids=list(range(n)))
    return np.stack([r["out"] for r in res.results], axis=0)


def kernel(**inputs):
    x = np.asarray(inputs["x"], np.float32)
    pv = pack_pv(inputs)
    gfin = np.ascontiguousarray(np.broadcast_to(np.asarray(inputs["norm_final"], np.float32)[None, :], (128, D)))
    if FUSED:
        nc = build(layers=(0, 1), final_norm=True)
        return _launch(nc, x, inputs, _WC + _W0 + _W1, pv, gfin)
    nc0 = build(layers=(0,), final_norm=False)
    x1 = _launch(nc0, x, inputs, _WC + _W0, pv, gfin)
    nc1 = build(layers=(1,), final_norm=True)
    return _launch(nc1, x1, inputs, _WC + _W1, pv, gfin)
```

```python
import numpy as np
from contextlib import ExitStack
import concourse.bass as bass
import concourse.mybir as mybir
from concourse.bass_utils import run_bass_kernel_spmd

F32 = mybir.dt.float32
BF16 = mybir.dt.bfloat16
AF = mybir.ActivationFunctionType
ALU = mybir.AluOpType

D = 1024
S = 2048
NT = 16
NB = 4
DFF = 3584
NG = 7
NE = 8
DIN = 2560
EPS = 1e-6
NRING = 6
GC0 = 0.7978845608028654
GC1 = 0.044715

PV_GMIX = 0
PV_GFFN = 8
PV_CONV = 16
PV_LCW = 28
PV_LCB = 44
PV_BA = 48
PV_BX = 52
PV_LAM = 56
PV_L = 60
PV_GFIN = 2 * PV_L
PV_ID = PV_GFIN + 8
PV_U = PV_ID + 128
PV_TOK = PV_U + 128
PV_IOTA = PV_TOK + 16
PV_N = PV_IOTA + 8
U32 = mybir.dt.uint32
I32 = mybir.dt.int32
JMAX = 5


class Tok:
    __slots__ = ("sem", "val")

    def __init__(self, sem, val):
        self.sem = sem
        self.val = val


class Buf:
    __slots__ = ("name", "w", "r")

    def __init__(self, name):
        self.name = name
        self.w = None
        self.r = {}


class DSem:
    __slots__ = ("h", "count")

    def __init__(self, h):
        self.h = h
        self.count = 0


class Eng:
    def __init__(self, name, sem):
        self.name = name
        self.sem = sem
        self.count = 0
        self.seen = {}
        self.ops = []


class Prog:
    ENGS = (("pe", "tensor"), ("act", "scalar"), ("dve", "vector"), ("pool", "gpsimd"), ("sp", "sync"))

    def __init__(self, nc, st):
        self.nc = nc
        self.st = st
        self.eng = {}
        for n, _ in self.ENGS:
            self.eng[n] = Eng(n, st.enter_context(nc.semaphore("prog_" + n)))
        self.nbuf = 0
        self.guard = None
        self.dsems = []
        self.regs = {}
        for n, attr in self.ENGS:
            eng = getattr(nc, attr)
            self.regs[n] = [st.enter_context(eng.register(f"rg_{n}{i}")) for i in range(2)]

    def buf(self, name="b"):
        self.nbuf += 1
        return Buf(f"{name}{self.nbuf}")

    def dsem(self, name):
        d = DSem(self.st.enter_context(self.nc.semaphore(name)))
        self.dsems.append(d)
        return d

    def regload(self, parity, ap, reads):
        for n, _ in self.ENGS:
            e = self.eng[n]
            waits = self._waits(e, reads, ())
            reg = self.regs[n][parity]
            e.ops.append((waits, [lambda eng, reg=reg: eng.reg_load(reg, ap)], None, None, None))

    def region_begin(self, parity, thr):
        self._saved_seen = {n: dict(e.seen) for n, e in self.eng.items()}
        d0 = {id(d.h): d.count for d in self.dsems}
        for n, _ in self.ENGS:
            self.eng[n].ops.append(("region_begin", parity, thr, d0))

    def region_end(self):
        for n, _ in self.ENGS:
            self.eng[n].ops.append(("region_end",))
            self.eng[n].seen = self._saved_seen[n]

    def barrier(self):
        toks = [Tok(e.sem, e.count) for e in self.eng.values() if e.count > 0]
        toks += [Tok(d.h, d.count) for d in self.dsems if d.count > 0]
        for n, _ in self.ENGS:
            self.wait(n, toks)

    def _waits(self, e, reads, writes, extra=()):
        need = {}

        def want(t):
            if t is None:
                return
            k = id(t.sem)
            if k not in need or need[k].val < t.val:
                need[k] = t
        for b in reads:
            want(b.w)
        for b in writes:
            want(b.w)
            for t in b.r.values():
                want(t)
        for t in extra:
            want(t)
        waits = []
        for k, t in need.items():
            if e.seen.get(k, 0) < t.val:
                e.seen[k] = t.val
                waits.append((t.sem, t.val))
        return waits

    def _commit(self, tok, reads, writes):
        k = id(tok.sem)
        for b in reads:
            if k not in b.r or b.r[k].val < tok.val:
                b.r[k] = tok
        for b in writes:
            b.w = tok
            b.r = {}

    def op(self, en, fn, reads=(), writes=(), dma=None, extra=(), skip=None):
        e = self.eng[en]
        waits = self._waits(e, reads, writes, extra)
        if dma is not None:
            dma.count += 16
            tok = Tok(dma.h, dma.count)
            inc = (dma.h, 16)
        else:
            e.count += 1
            tok = Tok(e.sem, e.count)
            inc = (e.sem, 1)
        e.ops.append((waits, [fn], inc, self.guard, skip))
        self._commit(tok, reads, writes)
        return tok

    def group(self, en, fns, reads=(), writes=(), skip=None):
        e = self.eng[en]
        waits = self._waits(e, reads, writes)
        e.count += 1
        tok = Tok(e.sem, e.count)
        e.ops.append((waits, list(fns), (e.sem, 1), self.guard, skip))
        self._commit(tok, reads, writes)
        return tok

    def wait(self, en, toks):
        e = self.eng[en]
        waits = self._waits(e, (), (), extra=toks)
        e.ops.append((waits, [], None, None, None))

    def emit(self):
        block = self.st.enter_context(self.nc.Block())
        for n, attr in self.ENGS:
            ops = self.eng[n].ops

            regs = self.regs[n]

            def emit_ops(e, ops, regs):
                i = 0
                while i < len(ops):
                    ent = ops[i]
                    if ent[0] == "region_begin":
                        depth, k = 1, i + 1
                        while depth:
                            if ops[k][0] == "region_begin":
                                depth += 1
                            elif ops[k][0] == "region_end":
                                depth -= 1
                            k += 1
                        inner = ops[i + 1:k - 1]
                        totals = {}
                        for o in inner:
                            if len(o) == 5 and o[2] is not None:
                                key = id(o[2][0])
                                totals[key] = (o[2][0], totals.get(key, (None, 0))[1] + o[2][1])
                        if any(len(o) == 5 and o[1] for o in inner):
                            with e.If_lt(regs[ent[1]], ent[2]):
                                for sem, tot in totals.values():
                                    if ent[3].get(id(sem), 0) > 0:
                                        e.wait_ge(sem, ent[3][id(sem)])
                                    while tot > 0:
                                        e.drain().then_inc(sem, min(tot, 4096))
                                        tot -= 4096
                            with e.Else():
                                emit_ops(e, inner, regs)
                        i = k
                        continue
                    waits, fns, inc, guard, skip = ent
                    for sem, val in waits:
                        e.wait_ge(sem, val)
                    i += 1
                    if not fns:
                        continue
                    if guard is None:
                        for fn in fns[:-1]:
                            fn(e)
                        ins = fns[-1](e)
                        if inc is not None:
                            ins.then_inc(inc[0], inc[1])
                    else:
                        par, thr = guard
                        with e.If_lt(regs[par], thr):
                            (skip(e) if skip is not None else e.drain()).then_inc(inc[0], inc[1])
                        with e.Else():
                            for fn in fns[:-1]:
                                fn(e)
                            fns[-1](e).then_inc(inc[0], inc[1])

            def body(e, ops=ops, regs=regs):
                emit_ops(e, ops, regs)
            getattr(block, attr)(body)


def build(layers=(0, 1), final_norm=True, n_experts=NE, ffn_groups=NG, sparse=True):
    nc = bass.Bass("TRN2", target_bir_lowering=False)
    x_d = nc.dram_tensor("x", [S, D], F32, kind="ExternalInput").ap()
    pv_d = nc.dram_tensor("pv", [128, PV_N], F32, kind="ExternalInput").ap()
    gfin_d = nc.dram_tensor("gfin", [128, D], F32, kind="ExternalInput").ap()
    w_in_d = nc.dram_tensor("w_in", [2, D, DIN], F32, kind="ExternalInput").ap()
    w_out_d = nc.dram_tensor("w_out", [2, D, D], F32, kind="ExternalInput").ap()
    wa_d = nc.dram_tensor("lru_wa", [2, 8, 64, 64], F32, kind="ExternalInput").ap()
    wx_d = nc.dram_tensor("lru_wx", [2, 8, 64, 64], F32, kind="ExternalInput").ap()
    if 0 in layers:
        fg_d = nc.dram_tensor("ffn_w_gate", [1, D, DFF], F32, kind="ExternalInput").ap()
        fu_d = nc.dram_tensor("ffn_w_up", [1, D, DFF], F32, kind="ExternalInput").ap()
        fd_d = nc.dram_tensor("ffn_w_down", [1, DFF, D], F32, kind="ExternalInput").ap()
    if 1 in layers:
        wr_d = nc.dram_tensor("w_router", [1, D, NE], F32, kind="ExternalInput").ap()
        mg_d = nc.dram_tensor("moe_w_gate", [1, NE, D, DFF], F32, kind="ExternalInput").ap()
        mu_d = nc.dram_tensor("moe_w_up", [1, NE, D, DFF], F32, kind="ExternalInput").ap()
        md_d = nc.dram_tensor("moe_w_down", [1, NE, DFF, D], F32, kind="ExternalInput").ap()
    out_d = nc.dram_tensor("out", [S, D], F32, kind="ExternalOutput").ap()
    sparse = sparse and (1 in layers)
    if sparse:
        H_d = nc.dram_tensor("h_scr", [S, D], BF16, kind="Internal").ap()
        Xs_d = nc.dram_tensor("xs_scr", [S, D], F32, kind="Internal").ap()
        Y_d = nc.dram_tensor("y_scr", [NE * S, D], F32, kind="Internal").ap()
        T2_d = nc.dram_tensor("t2_scr", [128 * 128, 2], U32, kind="Internal").ap()

    with ExitStack() as st:
        P = Prog(nc, st)

        def sb(name, shape, dt):
            return st.enter_context(nc.sbuf_tensor(name, shape, dt))

        xs = sb("xs", [128, NT, D], F32)
        hT = sb("hT", [128, 8, S], BF16)
        hTm = sb("hTm", [128, 8, 512], BF16)
        yT = sb("yT", [128, 8, 512], BF16)
        ring = [sb(f"ring{i}", [128, 8, 512], BF16) for i in range(NRING)]
        pv = sb("pvs", [128, PV_N], F32)
        ident = sb("ident", [128, 128], BF16)
        wbd = sb("wbd", [128, 8, 128], BF16)
        cc = sb("cc", [128, 16], F32)
        hbias = sb("hbias", [128, 16], F32)
        ss = sb("ss", [128, 16], F32)
        rs = sb("rs", [128, 16], F32)
        cvs = sb("cvs", [128, 514], F32)
        ups_ = [sb(f"ups{i}", [128, 515], F32) for i in range(2)]
        ctail = sb("ctail", [128, 4, 2], F32)
        utail = sb("utail", [128, 4, 3], F32)
        hlast = sb("hlast", [128, 4], F32)
        s32big = sb("s32big", [128, 12, 512], F32)
        s32 = [s32big[:, i, :] for i in range(12)]
        s16 = [sb(f"s16_{i}", [128, 1024], BF16) for i in range(4)]
        hid = [s16[1], s16[2]]
        hidT = [s16[0], s16[3]]
        HIDK = [("xn", 0), ("xn", 1)]
        HIDTK = [[("junk",)], [("ucb", 0), ("ucb", 1)]]
        if 1 in layers:
            xn32 = s32big[:, 0:2, :].rearrange("p a n -> p (a n)")
            xnT32 = s32big[:, 2:4, :].rearrange("p a (c t) -> p (a c) t", t=128)
            ident32 = sb("ident32", [128, 128], F32)
            wr_sb = sb("wr_sb", [128, 8, NE], F32)
            lg = sb("lg", [128, NE], F32)
            ex = sb("ex", [128, NE], F32)
            comb = sb("comb", [128, NT, NE], F32)
            m8 = sb("m8", [128, 8], F32)
            rsum = sb("rsum", [128, 2], F32)
        if sparse:
            msk = sb("msk", [128, NT, NE], F32)
            m1 = sb("m1", [128, NT, NE], F32)
            mskb = sb("mskb", [128, 128], BF16)
            Ubf = sb("Ubf", [128, 128], BF16)
            onesbf = sb("onesbf", [128, 128], BF16)
            nef = sb("nef", [128, NE], F32)
            cnt_i = sb("cnt_i", [128, NE], I32)
            PEG = sb("PEG", [128, 3, 32], F32)
            Ysel = sb("Ysel", [128, 32], U32)
            Tsc = sb("Tsc", [128, 32], U32)
            ti32 = sb("ti32", [128, 2, 32], I32)
            tokid = sb("tokid", [128, 16, 2], U32)
            idx_all = sb("idx_all", [128, 256], U32)
        pst = [st.enter_context(nc.psum_tensor(f"pst{i}", [128, 1024], BF16)) for i in range(2)]
        psf = [st.enter_context(nc.psum_tensor(f"psf{i}", [128, 512], F32)) for i in range(6)]

        B = {}

        def bf(*key):
            if key not in B:
                B[key] = P.buf(str(key))
            return B[key]

        junk_a = sb("junk_a", [128, 2], F32)
        junk_v = sb("junk_v", [128, 2], F32)

        def skip_for(en, out_ap):
            if P.guard is None:
                return None
            if en == "act":
                return lambda e: e.activation(out=junk_a[0:1, 0:1], in_=junk_a[0:1, 1:2], func=AF.Copy)
            if en == "dve":
                return lambda e: e.memset(junk_v[0:1, 0:1], 0.0)
            if en == "pe" and out_ap is not None:
                tgt = out_ap[0:1, 0:1] if out_ap.dtype == F32 else out_ap[0:1, 0:2].bitcast(F32)
                return lambda e: e.matmul(tgt, lhsT=ident[:, 0:1], rhs=ident[:, 0:1], start=True, stop=True)
            return None

        def O(en, meth, reads=(), writes=(), dma=None, **kw):
            return P.op(en, lambda e: getattr(e, meth)(**kw), reads, writes, dma, skip=skip_for(en, None))

        def G(en, calls, reads=(), writes=()):
            return P.group(en, [(lambda e, m=m, kw=kw: getattr(e, m)(**kw)) for m, kw in calls], reads, writes,
                           skip=skip_for(en, calls[0][1].get("out")))

        ring_sem = [P.dsem(f"ring_s{i}") for i in range(NRING)]
        pv_sem, wr_sem, wbd_sem, gf_sem = P.dsem("pv_s"), P.dsem("wr_s"), P.dsem("wbd_s"), P.dsem("gf_s")
        xl_sem = [P.dsem(f"xl{i}") for i in range(NB)]
        out_sem = P.dsem("outs")
        if sparse:
            xsp_sem, y_sem = P.dsem("xsp_s"), P.dsem("y_s")
            h_sem = [P.dsem(f"h_s{i}") for i in range(2)]
            xr_sem = [P.dsem(f"xr_s{i}") for i in range(NT)]
            t2_sem, sc_sem, ix_sem = P.dsem("t2_s"), P.dsem("sc_s"), P.dsem("ix_s")
            g_sem = [P.dsem(f"g_s{i}") for i in range(6)]
            yg_sem = [P.dsem(f"yg_s{i}") for i in range(6)]
        ring_pos = [0]

        def load_w(src_ap, split=False):
            i = ring_pos[0] % NRING
            ring_pos[0] += 1
            dst = ring[i][:].rearrange("p (c h) n -> p c h n", h=2) if split else ring[i][:]
            O("pool", "dma_start", writes=[bf("ring", i)], dma=ring_sem[i], out=dst, in_=src_ap)
            return i

        O("dve", "memset", writes=[bf("junk_a")], ap=junk_a[:], constant=0.0)
        O("dve", "memset", writes=[bf("junk_v")], ap=junk_v[:], constant=0.0)
        O("sp", "dma_start", writes=[bf("pv")], dma=pv_sem, out=pv[:], in_=pv_d)
        for b in range(NB):
            src = x_d[b * 512:(b + 1) * 512, :].rearrange("(i p) d -> p i d", p=128)
            O("sp", "dma_start", writes=[bf("xs", 4 * b + i) for i in range(4)], dma=xl_sem[b],
              out=xs[:, 4 * b:4 * b + 4, :], in_=src)
        O("act", "activation", reads=[bf("pv")], writes=[bf("ident")], out=ident[:], in_=pv[:, PV_ID:PV_ID + 128], func=AF.Copy)
        if 1 in layers:
            O("act", "activation", reads=[bf("pv")], writes=[bf("ident32")], out=ident32[:], in_=pv[:, PV_ID:PV_ID + 128], func=AF.Copy)
            O("sp", "dma_start", writes=[bf("wr_sb")], dma=wr_sem, out=wr_sb[:], in_=wr_d[0].rearrange("(k p) n -> p k n", p=128))
        if sparse:
            O("act", "activation", reads=[bf("pv")], writes=[bf("Ubf")], out=Ubf[:], in_=pv[:, PV_U:PV_U + 128], func=AF.Copy)
            O("dve", "memset", writes=[bf("onesbf")], ap=onesbf[:], constant=1.0)
            for c in range(2):
                O("dve", "tensor_copy", reads=[bf("pv")], writes=[bf("tokid")], out=tokid[:, :, c], in_=pv[:, PV_TOK:PV_TOK + 16])

        def build_wbd(l):
            O("dve", "memset", writes=[bf("wbd")], ap=wbd[:], constant=0.0)
            for g, wd_ in enumerate((wa_d, wx_d)):
                for q in range(2):
                    src = wd_[l].rearrange("(c q) i j -> q i c j", q=2)[q]
                    dst = wbd[q * 64:(q + 1) * 64, g * 4:g * 4 + 4, q * 64:(q + 1) * 64]
                    O("pool", "dma_start", writes=[bf("wbd")], dma=wbd_sem, out=dst, in_=src)

        for l in layers:
            lam = pv[:, l * PV_L + PV_LAM:l * PV_L + PV_LAM + 4]
            c1 = cc[:, l * 4:l * 4 + 4]
            c2 = cc[:, 8 + l * 4:8 + l * 4 + 4]
            O("act", "activation", reads=[bf("pv")], writes=[bf("cc")], out=c1, in_=lam, func=AF.Exp, scale=-1.0)
            O("act", "activation", reads=[bf("cc")], writes=[bf("cc")], out=c1, in_=c1, func=AF.Ln, bias=1.0)
            O("dve", "tensor_scalar", reads=[bf("cc")], writes=[bf("cc")], out=c2, in0=c1, scalar1=-16.0, scalar2=None, op0=ALU.mult)
            O("dve", "tensor_scalar", reads=[bf("cc")], writes=[bf("cc")], out=c1, in0=c1, scalar1=-8.0, scalar2=None, op0=ALU.mult)
            O("dve", "tensor_scalar", reads=[bf("pv")], writes=[bf("hbias")], out=hbias[:, l * 8:l * 8 + 8],
              in0=pv[:, l * PV_L + PV_BA:l * PV_L + PV_BA + 8], scalar1=-1.0, scalar2=None, op0=ALU.mult)

        def pvc(l, base, off=0, n=1):
            c0 = l * PV_L + base + off
            return pv[:, c0:c0 + n]

        def rstd_block(tis):
            t0, t1 = tis[0], tis[-1] + 1
            for ti in tis:
                O("act", "activation", reads=[bf("xs", ti)], writes=[bf("junk"), bf("ss", ti)], out=s16[0][:], in_=xs[:, ti, :],
                  func=AF.Square, accum_out=ss[:, ti:ti + 1])
            sk = [bf("ss", ti) for ti in tis]
            rk = [bf("rs", ti) for ti in tis]
            O("dve", "tensor_scalar", reads=sk, writes=rk, out=rs[:, t0:t1], in0=ss[:, t0:t1], scalar1=1.0 / D, scalar2=EPS,
              op0=ALU.mult, op1=ALU.add)
            O("act", "activation", reads=rk, writes=rk, out=rs[:, t0:t1], in_=rs[:, t0:t1], func=AF.Ln)
            O("act", "activation", reads=rk, writes=rk, out=rs[:, t0:t1], in_=rs[:, t0:t1], func=AF.Exp, scale=-0.5)

        def rstd(ti):
            rstd_block([ti])

        def rmsnorm_T(ti, gcol, dst, dst_c0, dst_buf, have_rstd=False, pb_i=0):
            xb = bf("xs", ti)
            if not have_rstd:
                rstd(ti)
            xn = s16[1 + (ti % 2)]
            xnb = bf("xn", ti % 2)
            O("act", "activation", reads=[xb, bf("rs", ti)], writes=[xnb], out=xn[:], in_=xs[:, ti, :], func=AF.Copy,
              scale=rs[:, ti:ti + 1])
            pb = bf("pst", pb_i)
            G("pe", [("transpose", dict(out=pst[pb_i][:, c * 128:(c + 1) * 128], in_=xn[:, c * 128:(c + 1) * 128], identity=ident[:]))
                     for c in range(8)], reads=[xnb, bf("ident")], writes=[pb])
            gb = gcol.unsqueeze(2).broadcast_to([128, 8, 128])
            src3 = pst[pb_i][:].rearrange("p (c t) -> p c t", c=8)
            O("dve", "tensor_tensor", reads=[pb, bf("pv")], writes=[dst_buf], out=dst[:, :, dst_c0:dst_c0 + 128], in0=src3, in1=gb,
              op=ALU.mult)

        def mm_calls(out_ap, pairs):
            n = len(pairs)
            return [("matmul", dict(out=out_ap, lhsT=lhsT, rhs=rhs, start=(i == 0), stop=(i == n - 1)))
                    for i, (lhsT, rhs) in enumerate(pairs)]

        def router(ti, l):
            if sparse:
                rxn32 = hT[:, 0, :].bitcast(F32)
                rxnT32 = hT[:, 1, :].bitcast(F32).rearrange("p (c t) -> p c t", t=128)
                RXK, RTK = [bf("hTscr", 0)], [bf("hTscr", 1), bf("hTscr", 2)]
                RX0 = RXK + [bf("hT", bb) for bb in range(NB)]
            else:
                rxn32, rxnT32 = xn32, xnT32
                RXK, RTK = [bf("s32", 0), bf("s32", 1)], [bf("s32", 2), bf("s32", 3)]
                RX0 = RXK
            xb = bf("xs", ti)
            O("act", "activation", reads=[xb, bf("rs", ti)], writes=RX0, out=rxn32[:], in_=xs[:, ti, :], func=AF.Copy,
              scale=rs[:, ti:ti + 1])
            yield
            rps = pst[0][:, 0:1024].bitcast(F32)
            RPK = bf("pst", 0)
            for hh in range(2):
                G("pe", [("transpose", dict(out=rps[:, c * 128:(c + 1) * 128],
                                            in_=rxn32[:, (4 * hh + c) * 128:(4 * hh + c + 1) * 128], identity=ident32[:]))
                         for c in range(4)], reads=RXK + [bf("ident32")], writes=[RPK])
                gb = pvc(l, PV_GFFN, 4 * hh, 4).unsqueeze(2).broadcast_to([128, 4, 128])
                O("dve", "tensor_tensor", reads=[RPK, bf("pv")], writes=[RTK[hh]],
                  out=rxnT32[:, 4 * hh:4 * hh + 4, :], in0=rps.rearrange("p (c t) -> p c t", c=4), in1=gb, op=ALU.mult)
                yield
            G("pe", mm_calls(rps[:, 0:NE], [(rxnT32[:, c, :], wr_sb[:, c, :]) for c in range(8)]),
              reads=RTK + [bf("wr_sb")], writes=[RPK])
            lgt = lg[:]
            cbt = comb[:, ti, :]
            O("act", "activation", reads=[RPK], writes=[bf("lg")], out=lgt, in_=rps[:, 0:NE], func=AF.Copy)
            yield
            O("dve", "scalar_tensor_tensor", reads=[bf("lg"), bf("pv")], writes=[bf("lg")], out=lgt, in0=pv[:, PV_IOTA:PV_IOTA + NE],
              scalar=-1e-30, in1=lgt, op0=ALU.mult, op1=ALU.add)
            O("dve", "max", reads=[bf("lg")], writes=[bf("m8")], out=m8[:], in_=lgt)
            O("dve", "tensor_scalar", reads=[bf("lg"), bf("m8")], writes=[bf("comb", ti)], out=cbt, in0=lgt, scalar1=m8[:, 1:2],
              scalar2=None, op0=ALU.is_ge)
            if sparse:
                O("dve", "tensor_copy", reads=[bf("comb", ti)], writes=[bf("msk", ti)], out=msk[:, ti, :], in_=cbt)
                O("dve", "tensor_scalar", reads=[bf("lg"), bf("m8")], writes=[bf("m1", ti)], out=m1[:, ti, :], in0=lgt,
                  scalar1=m8[:, 0:1], scalar2=None, op0=ALU.is_equal)
            O("dve", "tensor_scalar", reads=[bf("m8")], writes=[bf("rsum")], out=rsum[:, 0:1], in0=m8[:, 0:1], scalar1=-1.0,
              scalar2=None, op0=ALU.mult)
            O("act", "activation", reads=[bf("lg"), bf("rsum")], writes=[bf("ex")], out=ex[:], in_=lgt, func=AF.Exp,
              bias=rsum[:, 0:1], scale=1.0)
            O("dve", "tensor_tensor", reads=[bf("comb", ti), bf("ex")], writes=[bf("comb", ti)], out=cbt, in0=cbt, in1=ex[:], op=ALU.mult)
            O("dve", "reduce_sum", reads=[bf("comb", ti)], writes=[bf("rsum")], out=rsum[:, 1:2], in_=cbt, axis=mybir.AxisListType.X)
            O("dve", "reciprocal", reads=[bf("rsum")], writes=[bf("rsum")], out=rsum[:, 1:2], in_=rsum[:, 1:2])
            O("dve", "tensor_scalar", reads=[bf("comb", ti), bf("rsum")], writes=[bf("comb", ti)], out=cbt, in0=cbt,
              scalar1=rsum[:, 1:2], scalar2=None, op0=ALU.mult)
            yield

        def ffn(l):
            experts = [None] if l == 0 else list(range(n_experts))
            items = [(e_, g, ti) for e_ in experts for g in range(ffn_groups) for ti in range(NT)]
            slots = {}

            def stage_gu(n):
                e_, g, ti = items[n]
                if ti == 0:
                    if e_ is None:
                        sg_, su_, sd_ = fg_d[0], fu_d[0], fd_d[0]
                    else:
                        sg_, su_, sd_ = mg_d[0][e_], mu_d[0][e_], md_d[0][e_]
                    a = load_w(sg_[:, g * 512:(g + 1) * 512].rearrange("(k p) n -> p k n", p=128))
                    b_ = load_w(su_[:, g * 512:(g + 1) * 512].rearrange("(k p) n -> p k n", p=128))
                    c_ = load_w(sd_[g * 512:(g + 1) * 512, :].rearrange("(c p) (h n) -> p c h n", p=128, h=2), split=True)
                    slots[(e_, g)] = (a, b_, c_)
                a, b_, c_ = slots[(e_, g)]
                pg, pu = n % 2, 2 + n % 2
                hb = bf("hT", ti // 4)
                calls = []
                for k in range(8):
                    lhsT = hT[:, k, ti * 128:(ti + 1) * 128]
                    calls.append(("matmul", dict(out=psf[pg][:], lhsT=lhsT, rhs=ring[a][:, k, :], start=(k == 0), stop=(k == 7))))
                    calls.append(("matmul", dict(out=psf[pu][:], lhsT=lhsT, rhs=ring[b_][:, k, :], start=(k == 0), stop=(k == 7))))
                G("pe", calls, reads=[hb, bf("ring", a), bf("ring", b_)], writes=[bf("psf", pg), bf("psf", pu)])
                sil = s32[n % 2]
                O("act", "activation", reads=[bf("psf", pg)], writes=[bf("s32", n % 2)], out=sil[:], in_=psf[pg][:], func=AF.Silu)
                O("dve", "tensor_tensor", reads=[bf("s32", n % 2), bf("psf", pu)], writes=[bf(*HIDK[n % 2])], out=hid[n % 2][:, 0:512],
                  in0=sil[:], in1=psf[pu][:], op=ALU.mult)

            def stage_tr(n):
                pt = n % 2
                G("pe", [("transpose", dict(out=pst[pt][:, c * 128:(c + 1) * 128], in_=hid[n % 2][:, c * 128:(c + 1) * 128],
                                            identity=ident[:])) for c in range(4)],
                  reads=[bf(*HIDK[n % 2]), bf("ident")], writes=[bf("pst", pt)])
                O("act", "activation", reads=[bf("pst", pt)], writes=[bf(*k) for k in HIDTK[n % 2]], out=hidT[n % 2][:, 0:512], in_=pst[pt][:, 0:512],
                  func=AF.Copy)

            def stage_dn(n):
                e_, g, ti = items[n]
                c_ = slots[(e_, g)][2]
                xb = bf("xs", ti)
                for h in range(2):
                    bank = 4 + h
                    pairs = [(hidT[n % 2][:, c * 128:(c + 1) * 128], ring[c_][:, 2 * c + h, :]) for c in range(4)]
                    G("pe", mm_calls(psf[bank][:], pairs), reads=[bf(*k) for k in HIDTK[n % 2]] + [bf("ring", c_)], writes=[bf("psf", bank)])
                    xo = xs[:, ti, h * 512:(h + 1) * 512]
                    if e_ is None:
                        O("dve", "tensor_tensor", reads=[bf("psf", bank), xb], writes=[xb], out=xo, in0=psf[bank][:], in1=xo, op=ALU.add)
                    else:
                        O("dve", "scalar_tensor_tensor", reads=[bf("psf", bank), xb, bf("comb", ti)], writes=[xb], out=xo,
                          in0=psf[bank][:], scalar=comb[:, ti, e_:e_ + 1], in1=xo, op0=ALU.mult, op1=ALU.add)

            N = len(items)
            for s in range(N + 2):
                if s < N:
                    stage_gu(s)
                if 0 <= s - 1 < N:
                    stage_tr(s - 1)
                if 0 <= s - 2 < N:
                    stage_dn(s - 2)

        def moe_sparse(l):
            IOA = bass.IndirectOffsetOnAxis
            slots = {}
            for g in range(2):
                a = load_w(mg_d[0][0][:, g * 512:(g + 1) * 512].rearrange("(k p) n -> p k n", p=128))
                b_ = load_w(mu_d[0][0][:, g * 512:(g + 1) * 512].rearrange("(k p) n -> p k n", p=128))
                c_ = load_w(md_d[0][0][g * 512:(g + 1) * 512, :].rearrange("(c p) (h n) -> p c h n", p=128, h=2), split=True)
                slots[(0, g, "m")] = (a, b_, c_)
            P.barrier()
            f2 = lambda t: t[:].rearrange("p i e -> p (i e)")
            v3 = lambda a: a.rearrange("p (i e) -> p i e", e=NE)
            mskf, m1f, combf = f2(msk), f2(m1), f2(comb)
            K2, K3, K4, K5, K6 = (bf("s32", i) for i in (2, 3, 4, 5, 1))
            tot, off, pos, tmp, m2f = (s32[i][:, 0:128] for i in (2, 3, 4, 5, 1))
            allm = [bf("msk", i) for i in range(NT)] + [bf("m1", i) for i in range(NT)] + [bf("comb", i) for i in range(NT)]
            O("act", "activation", reads=allm, writes=[bf("mskb")], out=mskb[:], in_=mskf, func=AF.Copy)
            G("pe", [("matmul", dict(out=psf[0][:, 0:128], lhsT=Ubf[:], rhs=mskb[:], start=True, stop=True))],
              reads=[bf("Ubf"), bf("mskb")], writes=[bf("psf", 0)])
            G("pe", [("matmul", dict(out=psf[1][:, 0:128], lhsT=onesbf[:], rhs=mskb[:], start=True, stop=True))],
              reads=[bf("onesbf"), bf("mskb")], writes=[bf("psf", 1)])
            O("dve", "tensor_copy", reads=[bf("psf", 1)], writes=[K2], out=tot, in_=psf[1][:, 0:128])
            O("dve", "memset", writes=[K3], ap=off[:, 0:NE], constant=0.0)
            for i in range(1, NT):
                O("dve", "tensor_tensor", reads=[K2, K3], writes=[K3], out=off[:, NE * i:NE * i + NE], in0=off[:, NE * (i - 1):NE * i],
                  in1=tot[:, NE * (i - 1):NE * i], op=ALU.add)
            O("dve", "tensor_tensor", reads=[bf("psf", 0), K3], writes=[K4], out=pos, in0=psf[0][:, 0:128], in1=off, op=ALU.add)
            O("dve", "tensor_tensor", reads=[K2, K3], writes=[bf("nef")], out=nef[:], in0=off[:, 120:128], in1=tot[:, 120:128], op=ALU.add)
            O("dve", "tensor_copy", reads=[bf("nef")], writes=[bf("cnt")], out=cnt_i[:], in_=nef[:])
            O("dve", "tensor_tensor", reads=allm, writes=[K6], out=m2f, in0=mskf, in1=m1f, op=ALU.subtract)
            iota3 = pv[:, PV_IOTA:PV_IOTA + NE].unsqueeze(1).broadcast_to([128, NT, NE])
            for c, mc in ((0, m1f), (1, m2f)):
                for q, other in ((0, pos), (1, None), (2, combf)):
                    if other is None:
                        O("dve", "tensor_tensor", reads=allm + [K6, bf("pv")], writes=[K5], out=v3(tmp), in0=v3(mc), in1=iota3, op=ALU.mult)
                    else:
                        O("dve", "tensor_tensor", reads=allm + [K6, K4], writes=[K5], out=tmp, in0=mc, in1=other, op=ALU.mult)
                    O("dve", "reduce_sum", reads=[K5], writes=[bf("PEG")], out=PEG[:, q, 16 * c:16 * c + 16], in_=v3(tmp),
                      axis=mybir.AxisListType.X)
            Pc, Ec, Gc = PEG[:, 0, :], PEG[:, 1, :], PEG[:, 2, :]
            O("dve", "scalar_tensor_tensor", reads=[bf("PEG")], writes=[K5], out=tmp[:, 0:32], in0=Ec, scalar=float(S), in1=Pc,
              op0=ALU.mult, op1=ALU.add)
            O("dve", "tensor_scalar", reads=[K5], writes=[K5], out=tmp[:, 0:32], in0=tmp[:, 0:32], scalar1=float(NE * S - 1), scalar2=0.0,
              op0=ALU.min, op1=ALU.max)
            O("dve", "tensor_copy", reads=[K5], writes=[bf("Ysel")], out=Ysel[:], in_=tmp[:, 0:32])
            Pi, jj = ti32[:, 0, :], ti32[:, 1, :]
            jjf, t0 = tmp[:, 32:64], tmp[:, 64:96]
            O("dve", "tensor_copy", reads=[bf("PEG")], writes=[bf("ti32")], out=Pi, in_=Pc)
            O("dve", "tensor_scalar", reads=[bf("ti32")], writes=[bf("ti32")], out=jj, in0=Pi, scalar1=7, scalar2=None, op0=ALU.arith_shift_right)
            O("dve", "tensor_copy", reads=[bf("ti32")], writes=[K5], out=jjf, in_=jj)
            O("dve", "tensor_scalar", reads=[bf("PEG")], writes=[K5], out=t0, in0=Pc, scalar1=128.0, scalar2=None, op0=ALU.mult)
            O("dve", "scalar_tensor_tensor", reads=[K5], writes=[K5], out=t0, in0=jjf, scalar=-16383.0, in1=t0, op0=ALU.mult, op1=ALU.add)
            O("dve", "scalar_tensor_tensor", reads=[K5, bf("PEG")], writes=[K5], out=t0, in0=Ec, scalar=16.0, in1=t0, op0=ALU.mult, op1=ALU.add)
            O("dve", "tensor_scalar", reads=[K5], writes=[K5], out=t0, in0=t0, scalar1=float(128 * 128 - 1), scalar2=0.0, op0=ALU.min,
              op1=ALU.max)
            O("dve", "tensor_copy", reads=[K5], writes=[bf("Tsc")], out=Tsc[:], in_=t0)
            O("dve", "memset", writes=[bf("idx_all")], ap=idx_all[:], constant=0)
            O("sp", "dma_start", reads=[bf("idx_all")], writes=[bf("T2")], dma=t2_sem, out=T2_d.rearrange("(p a) c -> p (a c)", p=128),
              in_=idx_all[:])
            for c in range(2):
                for i in range(NT):
                    O("pool", "indirect_dma_start", reads=[bf("Tsc"), bf("tokid"), bf("T2")], writes=[bf("T2s", c, i)], dma=sc_sem, out=T2_d,
                      out_offset=IOA(ap=Tsc[:, 16 * c + i:16 * c + i + 1], axis=0), in_=tokid[:, i, :], in_offset=None)
            O("sp", "dma_start", reads=[bf("T2")] + [bf("T2s", c, i) for c in range(2) for i in range(NT)], writes=[bf("idx_all")],
              dma=ix_sem, out=idx_all[:],
              in_=T2_d.rearrange("(p a) c -> p (a c)", p=128))

            gffn3 = pvc(l, PV_GFFN, 0, 8).unsqueeze(2).broadcast_to([128, 8, 128])
            Hall = [bf("H", i) for i in range(NT)]
            npro = [0]

            def guard(e_, j):
                P.guard = (e_ % 2, 128 * j + 1)

            def prologue(e_, j):
                n = npro[0]
                npro[0] += 1
                if j == 0:
                    P.guard = None
                    P.regload(e_ % 2, cnt_i[0:1, e_:e_ + 1], [bf("cnt")])
                kk = bf("s32", 2 + n % 4)
                hg = s32big[:, 2 + n % 4, :].bitcast(BF16)
                col = 2 * (e_ * 16 + j)
                P.guard = None
                O("pool", "indirect_dma_start", reads=Hall + [bf("idx_all")], writes=[kk], dma=g_sem[n % 4], out=hg, out_offset=None,
                  in_=H_d, in_offset=IOA(ap=idx_all[:, col:col + 1], axis=0))
                guard(e_, j)
                pb = bf("pst", n % 2)
                G("pe", [("transpose", dict(out=pst[n % 2][:, c * 128:(c + 1) * 128], in_=hg[:, c * 128:(c + 1) * 128], identity=ident[:]))
                         for c in range(8)], reads=[kk, bf("ident")], writes=[pb])
                O("dve", "tensor_tensor", reads=[pb, bf("pv")], writes=[bf("hTj", j)], out=hT[:, :, j * 128:(j + 1) * 128],
                  in0=pst[n % 2][:].rearrange("p (c t) -> p c t", c=8), in1=gffn3, op=ALU.mult)
                P.guard = None

            def stage_gu(items, n, j0, tag):
                e_, g, j = items[n]
                if j == j0 and (e_, g, tag) not in slots:
                    a = load_w(mg_d[0][e_][:, g * 512:(g + 1) * 512].rearrange("(k p) n -> p k n", p=128))
                    b_ = load_w(mu_d[0][e_][:, g * 512:(g + 1) * 512].rearrange("(k p) n -> p k n", p=128))
                    c_ = load_w(md_d[0][e_][g * 512:(g + 1) * 512, :].rearrange("(c p) (h n) -> p c h n", p=128, h=2), split=True)
                    slots[(e_, g, tag)] = (a, b_, c_)
                a, b_, c_ = slots[(e_, g, tag)]
                pg, pu = n % 2, 2 + n % 2
                guard(e_, j)
                calls = []
                for k in range(8):
                    lhsT = hT[:, k, j * 128:(j + 1) * 128]
                    calls.append(("matmul", dict(out=psf[pg][:], lhsT=lhsT, rhs=ring[a][:, k, :], start=(k == 0), stop=(k == 7))))
                    calls.append(("matmul", dict(out=psf[pu][:], lhsT=lhsT, rhs=ring[b_][:, k, :], start=(k == 0), stop=(k == 7))))
                G("pe", calls, reads=[bf("hTj", j), bf("ring", a), bf("ring", b_)], writes=[bf("psf", pg), bf("psf", pu)])
                sil = s32[n % 2]
                P.guard = None
                O("act", "activation", reads=[bf("psf", pg)], writes=[bf("s32", n % 2)], out=sil[:], in_=psf[pg][:], func=AF.Silu)
                guard(e_, j)
                O("dve", "tensor_tensor", reads=[bf("s32", n % 2), bf("psf", pu)], writes=[bf(*HIDK[n % 2])], out=hid[n % 2][:, 0:512],
                  in0=sil[:], in1=psf[pu][:], op=ALU.mult)
                P.guard = None
                if tag == "m" and g == ffn_groups - 1 and e_ + 1 < n_experts:
                    prologue(e_ + 1, j)

            def stage_tr(items, n, j0, tag):
                e_, g, j = items[n]
                pt = n % 2
                guard(e_, j)
                G("pe", [("transpose", dict(out=pst[pt][:, c * 128:(c + 1) * 128], in_=hid[n % 2][:, c * 128:(c + 1) * 128],
                                            identity=ident[:])) for c in range(4)],
                  reads=[bf(*HIDK[n % 2]), bf("ident")], writes=[bf("pst", pt)])
                P.guard = None
                O("act", "activation", reads=[bf("pst", pt)], writes=[bf(*k) for k in HIDTK[n % 2]], out=hidT[n % 2][:, 0:512], in_=pst[pt][:, 0:512],
                  func=AF.Copy)

            def stage_dn(items, n, j0, tag):
                e_, g, j = items[n]
                c_ = slots[(e_, g, tag)][2]
                xb = bf("xs", j)
                guard(e_, j)
                for h in range(2):
                    bank = 4 + h
                    pairs = [(hidT[n % 2][:, c * 128:(c + 1) * 128], ring[c_][:, 2 * c + h, :]) for c in range(4)]
                    G("pe", mm_calls(psf[bank][:], pairs), reads=[bf(*k) for k in HIDTK[n % 2]] + [bf("ring", c_)], writes=[bf("psf", bank)])
                    xo = xs[:, j, h * 512:(h + 1) * 512]
                    if g == 0:
                        O("dve", "tensor_copy", reads=[bf("psf", bank)], writes=[xb], out=xo, in_=psf[bank][:])
                    else:
                        O("dve", "tensor_tensor", reads=[bf("psf", bank), xb], writes=[xb], out=xo, in0=psf[bank][:], in1=xo, op=ALU.add)
                P.guard = None
                if g == ffn_groups - 1:
                    r0 = e_ * S + j * 128
                    O("sp", "dma_start", reads=[xb], writes=[bf("Y")], dma=y_sem, out=Y_d[r0:r0 + 128, :], in_=xs[:, j, :])

            def run_items(items, j0, tag):
                N = len(items)
                for s_ in range(N + 2):
                    if s_ < N:
                        stage_gu(items, s_, j0, tag)
                    if 0 <= s_ - 1 < N:
                        stage_tr(items, s_ - 1, j0, tag)
                    if 0 <= s_ - 2 < N:
                        stage_dn(items, s_ - 2, j0, tag)

            for j in range(JMAX):
                prologue(0, j)
            run_items([(e_, g, j) for e_ in range(n_experts) for g in range(ffn_groups) for j in range(JMAX)], 0, "m")
            for e_ in range(n_experts):
                P.regload(e_ % 2, cnt_i[0:1, e_:e_ + 1], [bf("cnt")])
                P.region_begin(e_ % 2, 128 * JMAX + 1)
                for j in range(JMAX, NT):
                    prologue(e_, j)
                run_items([(e_, g, j) for g in range(ffn_groups) for j in range(JMAX, NT)], JMAX, "o")
                P.region_end()

            for i in range(NT):
                xb = bf("xs", i)
                O("sp", "dma_start", reads=[bf("Xs", t) for t in range(NT)], writes=[xb], dma=xr_sem[i], out=xs[:, i, :], in_=Xs_d[i * 128:(i + 1) * 128, :])
                for c in range(2):
                    n = 2 * i + c
                    q = n % 6
                    keys = [bf("s32", 2 * q), bf("s32", 2 * q + 1)]
                    yg = s32big[:, 2 * q:2 * q + 2, :].rearrange("p a n -> p (a n)")
                    O("pool", "indirect_dma_start", reads=[bf("Y"), bf("Ysel")], writes=keys, dma=yg_sem[q], out=yg, out_offset=None,
                      in_=Y_d, in_offset=IOA(ap=Ysel[:, 16 * c + i:16 * c + i + 1], axis=0))
                    O("dve", "scalar_tensor_tensor", reads=keys + [xb, bf("PEG")], writes=[xb], out=xs[:, i, :], in0=yg,
                      scalar=PEG[:, 2, 16 * c + i:16 * c + i + 1], in1=xs[:, i, :], op0=ALU.mult, op1=ALU.add)

        for l in layers:
            build_wbd(l)
            O("dve", "memset", writes=[bf("ctail", j) for j in range(4)], ap=ctail[:], constant=0.0)
            O("dve", "memset", writes=[bf("utail", j) for j in range(4)], ap=utail[:], constant=0.0)
            for j in range(4):
                O("dve", "memset", writes=[bf("hlast", j)], ap=hlast[:, j:j + 1], constant=0.0)
            def win_tile(t):
                return load_w(w_in_d[l][:, t * 512:(t + 1) * 512].rearrange("(k p) n -> p k n", p=128))

            def proj(slot_i, oc, bank):
                pairs = [(ring[slot_i][:, k, (oc % 4) * 128:(oc % 4) * 128 + 128], hTm[:, k, :]) for k in range(8)]
                G("pe", mm_calls(psf[bank][:], pairs), reads=[bf("ring", slot_i), bf("hTm")], writes=[bf("psf", bank)])

            def gen_A(b):
                yT_b = bf("yT")
                sc, sv, sbg = win_tile(0), win_tile(2), win_tile(1)
                yield
                for j in range(4):
                    k0, k1 = 0, 1
                    csb, acc = s32[k0], s32[k1]
                    K0, K1 = bf("s32", k0), bf("s32", k1)
                    cvb = bf("cvs")
                    proj(sc, j, 0)
                    yield
                    O("act", "activation", reads=[bf("psf", 0)], writes=[K0], out=csb[:], in_=psf[0][:], func=AF.Copy)
                    proj(sv, 8 + j, 1)
                    yield
                    O("act", "activation", reads=[bf("ctail", j)], writes=[cvb], out=cvs[:, 0:2], in_=ctail[:, j, :], func=AF.Copy)
                    O("dve", "tensor_tensor", reads=[K0, bf("psf", 1)], writes=[cvb], out=cvs[:, 2:514], in0=csb[:],
                      in1=psf[1][:], op=ALU.mult)
                    yield
                    O("dve", "tensor_scalar", reads=[cvb, bf("pv")], writes=[K1], out=acc[:], in0=cvs[:, 0:512],
                      scalar1=pvc(l, PV_CONV, j), scalar2=None, op0=ALU.mult)
                    yield
                    for k in (1, 2):
                        O("dve", "scalar_tensor_tensor", reads=[cvb, K1, bf("pv")], writes=[K1], out=acc[:],
                          in0=cvs[:, k:k + 512], scalar=pvc(l, PV_CONV, k * 4 + j), in1=acc[:], op0=ALU.mult, op1=ALU.add)
                        yield
                    O("act", "activation", reads=[cvb], writes=[bf("ctail", j)], out=ctail[:, j, :], in_=cvs[:, 512:514], func=AF.Copy)
                    proj(sbg, 4 + j, 0)
                    O("dve", "tensor_tensor", reads=[K1, bf("psf", 0)], writes=[yT_b], out=yT[:, j, :], in0=acc[:],
                      in1=psf[0][:], op=ALU.mult)
                    yield

            def gen_B(b, sid, su):
                yT_b = bf("yT")
                ku, kr, ki, ka = (2, 3, 4, 5) if sid == 0 else (8, 9, 10, 11)
                ups = ups_[sid]
                bu, bi = (3, 4) if sid == 0 else (5, 2)
                pu_, pr, pi_ = psf[bu][:], psf[bu][:], psf[bi][:]
                PUK, PRK, PIK = bf("psf", bu), bf("psf", bu), bf("psf", bi)
                KU, KR, KI, KA = bf("s32", ku), bf("s32", kr), bf("s32", ki), bf("s32", ka)
                yield
                for j in (sid, sid + 2):
                    uc, r_, i_, a_ = s32[ku], s32[kr], s32[ki], s32[ka]
                    ucb = s16[3][:, sid * 512:(sid + 1) * 512]
                    UCB = bf("ucb", sid)
                    ub = bf("ups", sid)
                    proj(su, 12 + j, bu)
                    yield
                    O("act", "activation", reads=[bf("utail", j)], writes=[ub], out=ups[:, 0:3], in_=utail[:, j, :], func=AF.Copy)
                    O("act", "activation", reads=[PUK], writes=[ub], out=ups[:, 3:515], in_=pu_, func=AF.Copy)
                    yield
                    O("dve", "tensor_scalar", reads=[ub, bf("pv")], writes=[KU], out=uc[:], in0=ups[:, 0:512],
                      scalar1=pvc(l, PV_LCW, j), scalar2=pvc(l, PV_LCB, j), op0=ALU.mult, op1=ALU.add)
                    yield
                    for k in (1, 2, 3):
                        O("dve", "scalar_tensor_tensor", reads=[ub, KU, bf("pv")], writes=[KU], out=uc[:],
                          in0=ups[:, k:k + 512], scalar=pvc(l, PV_LCW, k * 4 + j), in1=uc[:], op0=ALU.mult, op1=ALU.add)
                        yield
                    O("act", "activation", reads=[ub], writes=[bf("utail", j)], out=utail[:, j, :], in_=ups[:, 512:515], func=AF.Copy)
                    O("act", "activation", reads=[KU], writes=[UCB], out=ucb, in_=uc[:], func=AF.Copy)
                    yield
                    G("pe", [("matmul", dict(out=pr, lhsT=wbd[:, j, :], rhs=ucb, start=True, stop=True))],
                      reads=[bf("wbd"), UCB], writes=[PRK])
                    G("pe", [("matmul", dict(out=pi_, lhsT=wbd[:, 4 + j, :], rhs=ucb, start=True, stop=True))],
                      reads=[bf("wbd"), UCB], writes=[PIK])
                    yield
                    for (t_, ps_, PK_, K_, hb_) in ((r_, pr, PRK, KR, l * 8 + j), (i_, pi_, PIK, KI, l * 8 + 4 + j)):
                        O("act", "activation", reads=[PK_, bf("hbias")], writes=[K_], out=t_[:], in_=ps_,
                          func=AF.Exp, bias=hbias[:, hb_:hb_ + 1], scale=-1.0)
                        yield
                        O("act", "activation", reads=[K_], writes=[K_], out=t_[:], in_=t_[:], func=AF.Ln, bias=1.0)
                        yield
                        O("act", "activation", reads=[K_], writes=[K_], out=t_[:], in_=t_[:], func=AF.Exp, scale=-1.0)
                        yield
                    O("dve", "tensor_tensor", reads=[KI, KU], writes=[KI], out=i_[:], in0=i_[:], in1=uc[:],
                      op=ALU.mult)
                    O("act", "activation", reads=[KR, bf("cc")], writes=[KA], out=a_[:], in_=r_[:], func=AF.Exp,
                      scale=cc[:, l * 4 + j:l * 4 + j + 1])
                    yield
                    O("act", "activation", reads=[KR, bf("cc")], writes=[KR], out=r_[:], in_=r_[:], func=AF.Exp,
                      scale=cc[:, 8 + l * 4 + j:8 + l * 4 + j + 1])
                    yield
                    O("act", "activation", reads=[KR], writes=[KR], out=r_[:], in_=r_[:], func=AF.Ln, bias=1.0000001, scale=-1.0)
                    yield
                    O("act", "activation", reads=[KR], writes=[KR], out=r_[:], in_=r_[:], func=AF.Exp, scale=0.5)
                    yield
                    O("dve", "tensor_tensor", reads=[KI, KR], writes=[KI], out=i_[:], in0=i_[:], in1=r_[:],
                      op=ALU.mult)
                    yield
                    hlb = bf("hlast", j)
                    O("dve", "tensor_tensor_scan", reads=[KA, KI, hlb], writes=[KU], out=uc[:],
                      data0=a_[:], data1=i_[:], initial=hlast[:, j:j + 1], op0=ALU.mult, op1=ALU.add)
                    yield
                    O("act", "activation", reads=[KU], writes=[hlb], out=hlast[:, j:j + 1], in_=uc[:, 511:512], func=AF.Copy)
                    yield ("wait", ("gelu", b, j))
                    O("dve", "tensor_tensor", reads=[bf("s32", 6 + j % 2), KU], writes=[yT_b], out=yT[:, 4 + j, :],
                      in0=s32[6 + j % 2][:], in1=uc[:], op=ALU.mult)
                    yield ("signal", ("yb", b, j))

            def gen_C(b):
                sg = win_tile(4)
                gps = pst[1][:, 0:1024].bitcast(F32)
                yield
                for j in range(4):
                    qk = 6 + j % 2
                    q_, QK = s32[qk], bf("s32", qk)
                    if j >= 2:
                        yield ("wait", ("yb", b, j - 2))
                    pairs = [(ring[sg][:, k, j * 128:j * 128 + 128], hTm[:, k, :]) for k in range(8)]
                    G("pe", mm_calls(gps, pairs), reads=[bf("ring", sg), bf("hTm")], writes=[bf("pst", 1)])
                    yield
                    O("act", "activation", reads=[bf("pst", 1)], writes=[QK], out=q_[:], in_=gps, func=AF.Square)
                    yield
                    O("dve", "tensor_scalar", reads=[QK], writes=[QK], out=q_[:], in0=q_[:], scalar1=GC1, scalar2=1.0,
                      op0=ALU.mult, op1=ALU.add)
                    yield
                    O("dve", "tensor_tensor", reads=[QK, bf("pst", 1)], writes=[QK], out=q_[:], in0=q_[:], in1=gps, op=ALU.mult)
                    yield
                    O("act", "activation", reads=[QK], writes=[QK], out=q_[:], in_=q_[:], func=AF.Exp, scale=-2.0 * GC0)
                    yield
                    O("act", "activation", reads=[QK], writes=[QK], out=q_[:], in_=q_[:], func=AF.Ln, bias=1.0)
                    yield
                    O("act", "activation", reads=[QK], writes=[QK], out=q_[:], in_=q_[:], func=AF.Exp, scale=-1.0)
                    yield
                    O("dve", "tensor_tensor", reads=[QK, bf("pst", 1)], writes=[QK], out=q_[:], in0=q_[:], in1=gps, op=ALU.mult)
                    yield ("signal", ("gelu", b, j))

            def tail_wout(b):
                yT_b = bf("yT")
                so = [load_w(w_out_d[l][:, h * 512:(h + 1) * 512].rearrange("(k p) n -> p k n", p=128)) for h in range(2)]
                for i in range(4):
                    ti = 4 * b + i
                    xb = bf("xs", ti)
                    for h in range(2):
                        bank = 1 + h
                        pairs = [(yT[:, k, i * 128:(i + 1) * 128], ring[so[h]][:, k, :]) for k in range(8)]
                        G("pe", mm_calls(psf[bank][:], pairs), reads=[yT_b, bf("ring", so[h])], writes=[bf("psf", bank)])
                        xo = xs[:, ti, h * 512:(h + 1) * 512]
                        O("dve", "tensor_tensor", reads=[bf("psf", bank), xb], writes=[xb], out=xo, in0=psf[bank][:], in1=xo, op=ALU.add)

            def gen_tail(b):
                yield
                rstd_block([4 * b + i for i in range(4)])
                yield
                for i in range(4):
                    ti = 4 * b + i
                    if l == 1 and sparse:
                        xb = bf("xs", ti)
                        xn = s16[1 + (ti % 2)]
                        xnb = bf("xn", ti % 2)
                        O("act", "activation", reads=[xb, bf("rs", ti)], writes=[xnb], out=xn[:], in_=xs[:, ti, :], func=AF.Copy,
                          scale=rs[:, ti:ti + 1])
                        O("sp", "dma_start", reads=[xnb], writes=[bf("H", ti)], dma=h_sem[ti % 2], out=H_d[ti * 128:(ti + 1) * 128, :], in_=xn[:])
                        yield
                        yield from router(ti, l)
                        O("sp", "dma_start", reads=[xb], writes=[bf("Xs", ti)], dma=xsp_sem, out=Xs_d[ti * 128:(ti + 1) * 128, :],
                          in_=xs[:, ti, :])
                        yield
                    else:
                        rmsnorm_T(ti, pvc(l, PV_GFFN, 0, 8), hT, ti * 128, bf("hT", b), have_rstd=True)
                        yield
                        if l == 1:
                            yield from router(ti, l)

            def run_streams(gens):
                gens = list(gens)
                blocked, done = {}, set()
                while gens:
                    progressed = False
                    for g_ in list(gens):
                        if id(g_) in blocked:
                            if blocked[id(g_)] not in done:
                                continue
                            del blocked[id(g_)]
                        progressed = True
                        try:
                            r = next(g_)
                        except StopIteration:
                            gens.remove(g_)
                            continue
                        if r is not None:
                            if r[0] == "signal":
                                done.add(r[1])
                            elif r[1] not in done:
                                blocked[id(g_)] = r[1]
                    assert progressed, "stream deadlock"

            for b in range(NB):
                rstd_block([4 * b + i for i in range(4)])
                for i in range(4):
                    rmsnorm_T(4 * b + i, pvc(l, PV_GMIX, 0, 8), hTm, i * 128, bf("hTm"), have_rstd=True, pb_i=i % 2)
                su_ = win_tile(3)
                run_streams([gen_B(b, 0, su_), gen_B(b, 1, su_), gen_C(b), gen_A(b)] + ([gen_tail(b - 1)] if b > 0 else []))
                tail_wout(b)
            run_streams([gen_tail(NB - 1)])
            if l == 1 and sparse:
                moe_sparse(l)
            else:
                ffn(l)

        otoks = []
        if final_norm:
            gfin = xn32
            O("sp", "dma_start", writes=[bf("s32", 0), bf("s32", 1)], dma=gf_sem, out=xn32[:], in_=gfin_d)
        if final_norm:
            rstd_block(list(range(NT)))
        for ti in range(NT):
            xb = bf("xs", ti)
            if final_norm:
                O("dve", "scalar_tensor_tensor", reads=[xb, bf("rs", ti), bf("s32", 0), bf("s32", 1)], writes=[xb], out=xs[:, ti, :], in0=xs[:, ti, :],
                  scalar=rs[:, ti:ti + 1], in1=gfin[:], op0=ALU.mult, op1=ALU.mult)
            otoks.append(O("sp", "dma_start", reads=[xb], dma=out_sem, out=out_d[ti * 128:(ti + 1) * 128, :], in_=xs[:, ti, :]))
        P.wait("sp", otoks)
        P.emit()
    return nc


def pack_pv(inp):
    pv = np.zeros((128, PV_N), np.float32)

    def col(v):
        return np.asarray(v, np.float32).reshape(-1, 128).T

    for l in range(2):
        o = l * PV_L
        pv[:, o + PV_GMIX:o + PV_GMIX + 8] = col(inp["norm_mix"][l])
        pv[:, o + PV_GFFN:o + PV_GFFN + 8] = col(inp["norm_ffn"][l])
        pv[:, o + PV_CONV:o + PV_CONV + 12] = np.asarray(inp["conv_w"][l], np.float32).reshape(3, 4, 128).transpose(2, 0, 1).reshape(128, 12)
        pv[:, o + PV_LCW:o + PV_LCW + 16] = np.asarray(inp["lru_conv_w"][l], np.float32).reshape(4, 4, 128).transpose(2, 0, 1).reshape(128, 16)
        pv[:, o + PV_LCB:o + PV_LCB + 4] = col(inp["lru_conv_b"][l])
        pv[:, o + PV_BA:o + PV_BA + 4] = col(inp["lru_ba"][l])
        pv[:, o + PV_BX:o + PV_BX + 4] = col(inp["lru_bx"][l])
        pv[:, o + PV_LAM:o + PV_LAM + 4] = col(inp["lru_lambda"][l])
    pv[:, PV_GFIN:PV_GFIN + 8] = col(inp["norm_final"])
    pv[:, PV_ID:PV_ID + 128] = np.eye(128, dtype=np.float32)
    pv[:, PV_U:PV_U + 128] = np.triu(np.ones((128, 128), np.float32), k=1)
    pv[:, PV_TOK:PV_TOK + 16] = (128.0 * np.arange(16)[None, :] + np.arange(128)[:, None]).astype(np.float32)
    pv[:, PV_IOTA:PV_IOTA + 8] = np.arange(8, dtype=np.float32)[None, :]
    return pv


_W0 = ("ffn_w_gate", "ffn_w_up", "ffn_w_down")
_W1 = ("w_router", "moe_w_gate", "moe_w_up", "moe_w_down")
_WC = ("w_in", "w_out", "lru_wa", "lru_wx")
FUSED = True


def _launch(nc, x, inp, names, pv, gfin):
    n = x.shape[0]
    shared = {k: np.ascontiguousarray(np.asarray(inp[k], np.float32)) for k in names}
    in_maps = []
    for c in range(n):
        m = {"x": np.ascontiguousarray(x[c]), "pv": pv, "gfin": gfin}
        m.update(shared)
        in_maps.append(m)
    res = run_bass_kernel_spmd(nc, in_maps, core_ids=list(range(n)))
    return np.stack([r["out"] for r in res.results], axis=0)


def kernel(**inputs):
    x = np.asarray(inputs["x"], np.float32)
    pv = pack_pv(inputs)
    gfin = np.ascontiguousarray(np.broadcast_to(np.asarray(inputs["norm_final"], np.float32)[None, :], (128, D)))
    if FUSED:
        nc = build(layers=(0, 1), final_norm=True)
        return _launch(nc, x, inputs, _WC + _W0 + _W1, pv, gfin)
    nc0 = build(layers=(0,), final_norm=False)
    x1 = _launch(nc0, x, inputs, _WC + _W0, pv, gfin)
    nc1 = build(layers=(1,), final_norm=True)
    return _launch(nc1, x1, inputs, _WC + _W1, pv, gfin)
```

```python
import numpy as np
from contextlib import ExitStack
import concourse.bass as bass
import concourse.mybir as mybir
from concourse.bass_utils import run_bass_kernel_spmd

F32 = mybir.dt.float32
BF16 = mybir.dt.bfloat16
AF = mybir.ActivationFunctionType
ALU = mybir.AluOpType

D = 1024
S = 2048
NT = 16
NB = 4
DFF = 3584
NG = 7
NE = 8
DIN = 2560
EPS = 1e-6
NRING = 6
GC0 = 0.7978845608028654
GC1 = 0.044715

PV_GMIX = 0
PV_GFFN = 8
PV_CONV = 16
PV_LCW = 28
PV_LCB = 44
PV_BA = 48
PV_BX = 52
PV_LAM = 56
PV_L = 60
PV_GFIN = 2 * PV_L
PV_ID = PV_GFIN + 8
PV_U = PV_ID + 128
PV_TOK = PV_U + 128
PV_IOTA = PV_TOK + 16
PV_N = PV_IOTA + 8
U32 = mybir.dt.uint32
I32 = mybir.dt.int32
JMAX = 5


class Tok:
    __slots__ = ("sem", "val")

    def __init__(self, sem, val):
        self.sem = sem
        self.val = val


class Buf:
    __slots__ = ("name", "w", "r")

    def __init__(self, name):
        self.name = name
        self.w = None
        self.r = {}


class DSem:
    __slots__ = ("h", "count")

    def __init__(self, h):
        self.h = h
        self.count = 0


class Eng:
    def __init__(self, name, sem):
        self.name = name
        self.sem = sem
        self.count = 0
        self.seen = {}
        self.ops = []


class Prog:
    ENGS = (("pe", "tensor"), ("act", "scalar"), ("dve", "vector"), ("pool", "gpsimd"), ("sp", "sync"))

    def __init__(self, nc, st):
        self.nc = nc
        self.st = st
        self.eng = {}
        for n, _ in self.ENGS:
            self.eng[n] = Eng(n, st.enter_context(nc.semaphore("prog_" + n)))
        self.nbuf = 0
        self.guard = None
        self.dsems = []
        self.regs = {}
        for n, attr in self.ENGS:
            eng = getattr(nc, attr)
            self.regs[n] = [st.enter_context(eng.register(f"rg_{n}{i}")) for i in range(2)]

    def buf(self, name="b"):
        self.nbuf += 1
        return Buf(f"{name}{self.nbuf}")

    def dsem(self, name):
        d = DSem(self.st.enter_context(self.nc.semaphore(name)))
        self.dsems.append(d)
        return d

    def regload(self, parity, ap, reads):
        for n, _ in self.ENGS:
            e = self.eng[n]
            waits = self._waits(e, reads, ())
            reg = self.regs[n][parity]
            e.ops.append((waits, [lambda eng, reg=reg: eng.reg_load(reg, ap)], None, None, None))

    def region_begin(self, parity, thr):
        self._saved_seen = {n: dict(e.seen) for n, e in self.eng.items()}
        d0 = {id(d.h): d.count for d in self.dsems}
        for n, _ in self.ENGS:
            self.eng[n].ops.append(("region_begin", parity, thr, d0))

    def region_end(self):
        for n, _ in self.ENGS:
            self.eng[n].ops.append(("region_end",))
            self.eng[n].seen = self._saved_seen[n]

    def barrier(self):
        toks = [Tok(e.sem, e.count) for e in self.eng.values() if e.count > 0]
        toks += [Tok(d.h, d.count) for d in self.dsems if d.count > 0]
        for n, _ in self.ENGS:
            self.wait(n, toks)

    def _waits(self, e, reads, writes, extra=()):
        need = {}

        def want(t):
            if t is None:
                return
            k = id(t.sem)
            if k not in need or need[k].val < t.val:
                need[k] = t
        for b in reads:
            want(b.w)
        for b in writes:
            want(b.w)
            for t in b.r.values():
                want(t)
        for t in extra:
            want(t)
        waits = []
        for k, t in need.items():
            if e.seen.get(k, 0) < t.val:
                e.seen[k] = t.val
                waits.append((t.sem, t.val))
        return waits

    def _commit(self, tok, reads, writes):
        k = id(tok.sem)
        for b in reads:
            if k not in b.r or b.r[k].val < tok.val:
                b.r[k] = tok
        for b in writes:
            b.w = tok
            b.r = {}

    def op(self, en, fn, reads=(), writes=(), dma=None, extra=(), skip=None):
        e = self.eng[en]
        waits = self._waits(e, reads, writes, extra)
        if dma is not None:
            dma.count += 16
            tok = Tok(dma.h, dma.count)
            inc = (dma.h, 16)
        else:
            e.count += 1
            tok = Tok(e.sem, e.count)
            inc = (e.sem, 1)
        e.ops.append((waits, [fn], inc, self.guard, skip))
        self._commit(tok, reads, writes)
        return tok

    def group(self, en, fns, reads=(), writes=(), skip=None):
        e = self.eng[en]
        waits = self._waits(e, reads, writes)
        e.count += 1
        tok = Tok(e.sem, e.count)
        e.ops.append((waits, list(fns), (e.sem, 1), self.guard, skip))
        self._commit(tok, reads, writes)
        return tok

    def wait(self, en, toks):
        e = self.eng[en]
        waits = self._waits(e, (), (), extra=toks)
        e.ops.append((waits, [], None, None, None))

    def emit(self):
        block = self.st.enter_context(self.nc.Block())
        for n, attr in self.ENGS:
            ops = self.eng[n].ops

            regs = self.regs[n]

            def emit_ops(e, ops, regs):
                i = 0
                while i < len(ops):
                    ent = ops[i]
                    if ent[0] == "region_begin":
                        depth, k = 1, i + 1
                        while depth:
                            if ops[k][0] == "region_begin":
                                depth += 1
                            elif ops[k][0] == "region_end":
                                depth -= 1
                            k += 1
                        inner = ops[i + 1:k - 1]
                        totals = {}
                        for o in inner:
                            if len(o) == 5 and o[2] is not None:
                                key = id(o[2][0])
                                totals[key] = (o[2][0], totals.get(key, (None, 0))[1] + o[2][1])
                        if any(len(o) == 5 and o[1] for o in inner):
                            with e.If_lt(regs[ent[1]], ent[2]):
                                for sem, tot in totals.values():
                                    if ent[3].get(id(sem), 0) > 0:
                                        e.wait_ge(sem, ent[3][id(sem)])
                                    while tot > 0:
                                        e.drain().then_inc(sem, min(tot, 4096))
                                        tot -= 4096
                            with e.Else():
                                emit_ops(e, inner, regs)
                        i = k
                        continue
                    waits, fns, inc, guard, skip = ent
                    for sem, val in waits:
                        e.wait_ge(sem, val)
                    i += 1
                    if not fns:
                        continue
                    if guard is None:
                        for fn in fns[:-1]:
                            fn(e)
                        ins = fns[-1](e)
                        if inc is not None:
                            ins.then_inc(inc[0], inc[1])
                    else:
                        par, thr = guard
                        with e.If_lt(regs[par], thr):
                            (skip(e) if skip is not None else e.drain()).then_inc(inc[0], inc[1])
                        with e.Else():
                            for fn in fns[:-1]:
                                fn(e)
                            fns[-1](e).then_inc(inc[0], inc[1])

            def body(e, ops=ops, regs=regs):
                emit_ops(e, ops, regs)
            getattr(block, attr)(body)


def build(layers=(0, 1), final_norm=True, n_experts=NE, ffn_groups=NG, sparse=True):
    nc = bass.Bass("TRN2", target_bir_lowering=False)
    x_d = nc.dram_tensor("x", [S, D], F32, kind="ExternalInput").ap()
    pv_d = nc.dram_tensor("pv", [128, PV_N], F32, kind="ExternalInput").ap()
    gfin_d = nc.dram_tensor("gfin", [128, D], F32, kind="ExternalInput").ap()
    w_in_d = nc.dram_tensor("w_in", [2, D, DIN], F32, kind="ExternalInput").ap()
    w_out_d = nc.dram_tensor("w_out", [2, D, D], F32, kind="ExternalInput").ap()
    wa_d = nc.dram_tensor("lru_wa", [2, 8, 64, 64], F32, kind="ExternalInput").ap()
    wx_d = nc.dram_tensor("lru_wx", [2, 8, 64, 64], F32, kind="ExternalInput").ap()
    if 0 in layers:
        fg_d = nc.dram_tensor("ffn_w_gate", [1, D, DFF], F32, kind="ExternalInput").ap()
        fu_d = nc.dram_tensor("ffn_w_up", [1, D, DFF], F32, kind="ExternalInput").ap()
        fd_d = nc.dram_tensor("ffn_w_down", [1, DFF, D], F32, kind="ExternalInput").ap()
    if 1 in layers:
        wr_d = nc.dram_tensor("w_router", [1, D, NE], F32, kind="ExternalInput").ap()
        mg_d = nc.dram_tensor("moe_w_gate", [1, NE, D, DFF], F32, kind="ExternalInput").ap()
        mu_d = nc.dram_tensor("moe_w_up", [1, NE, D, DFF], F32, kind="ExternalInput").ap()
        md_d = nc.dram_tensor("moe_w_down", [1, NE, DFF, D], F32, kind="ExternalInput").ap()
    out_d = nc.dram_tensor("out", [S, D], F32, kind="ExternalOutput").ap()
    sparse = sparse and (1 in layers)
    if sparse:
        H_d = nc.dram_tensor("h_scr", [S, D], BF16, kind="Internal").ap()
        Xs_d = nc.dram_tensor("xs_scr", [S, D], F32, kind="Internal").ap()
        Y_d = nc.dram_tensor("y_scr", [NE * S, D], F32, kind="Internal").ap()
        T2_d = nc.dram_tensor("t2_scr", [128 * 128, 2], U32, kind="Internal").ap()

    with ExitStack() as st:
        P = Prog(nc, st)

        def sb(name, shape, dt):
            return st.enter_context(nc.sbuf_tensor(name, shape, dt))

        xs = sb("xs", [128, NT, D], F32)
        hT = sb("hT", [128, 8, S], BF16)
        hTm = sb("hTm", [128, 8, 512], BF16)
        yT = sb("yT", [128, 8, 512], BF16)
        ring = [sb(f"ring{i}", [128, 8, 512], BF16) for i in range(NRING)]
        pv = sb("pvs", [128, PV_N], F32)
        ident = sb("ident", [128, 128], BF16)
        wbd = sb("wbd", [128, 8, 128], BF16)
        cc = sb("cc", [128, 16], F32)
        hbias = sb("hbias", [128, 16], F32)
        ss = sb("ss", [128, 16], F32)
        rs = sb("rs", [128, 16], F32)
        cvs = sb("cvs", [128, 514], F32)
        ups_ = [sb(f"ups{i}", [128, 515], F32) for i in range(2)]
        ctail = sb("ctail", [128, 4, 2], F32)
        utail = sb("utail", [128, 4, 3], F32)
        hlast = sb("hlast", [128, 4], F32)
        s32big = sb("s32big", [128, 12, 512], F32)
        s32 = [s32big[:, i, :] for i in range(12)]
        s16 = [sb(f"s16_{i}", [128, 1024], BF16) for i in range(4)]
        hid = [s16[1], s16[2]]
        hidT = [s16[0], s16[3]]
        HIDK = [("xn", 0), ("xn", 1)]
        HIDTK = [[("junk",)], [("ucb", 0), ("ucb", 1)]]
        if 1 in layers:
            xn32 = s32big[:, 0:2, :].rearrange("p a n -> p (a n)")
            xnT32 = s32big[:, 2:4, :].rearrange("p a (c t) -> p (a c) t", t=128)
            ident32 = sb("ident32", [128, 128], F32)
            wr_sb = sb("wr_sb", [128, 8, NE], F32)
            lg = sb("lg", [128, NE], F32)
            ex = sb("ex", [128, NE], F32)
            comb = sb("comb", [128, NT, NE], F32)
            m8 = sb("m8", [128, 8], F32)
            rsum = sb("rsum", [128, 2], F32)
        if sparse:
            msk = sb("msk", [128, NT, NE], F32)
            m1 = sb("m1", [128, NT, NE], F32)
            mskb = sb("mskb", [128, 128], BF16)
            Ubf = sb("Ubf", [128, 128], BF16)
            onesbf = sb("onesbf", [128, 128], BF16)
            nef = sb("nef", [128, NE], F32)
            cnt_i = sb("cnt_i", [128, NE], I32)
            PEG = sb("PEG", [128, 3, 32], F32)
            Ysel = sb("Ysel", [128, 32], U32)
            Tsc = sb("Tsc", [128, 32], U32)
            ti32 = sb("ti32", [128, 2, 32], I32)
            tokid = sb("tokid", [128, 16, 2], U32)
            idx_all = sb("idx_all", [128, 256], U32)
        pst = [st.enter_context(nc.psum_tensor(f"pst{i}", [128, 1024], BF16)) for i in range(2)]
        psf = [st.enter_context(nc.psum_tensor(f"psf{i}", [128, 512], F32)) for i in range(6)]

        B = {}

        def bf(*key):
            if key not in B:
                B[key] = P.buf(str(key))
            return B[key]

        junk_a = sb("junk_a", [128, 2], F32)
        junk_v = sb("junk_v", [128, 2], F32)

        def skip_for(en, out_ap):
            if P.guard is None:
                return None
            if en == "act":
                return lambda e: e.activation(out=junk_a[0:1, 0:1], in_=junk_a[0:1, 1:2], func=AF.Copy)
            if en == "dve":
                return lambda e: e.memset(junk_v[0:1, 0:1], 0.0)
            if en == "pe" and out_ap is not None:
                tgt = out_ap[0:1, 0:1] if out_ap.dtype == F32 else out_ap[0:1, 0:2].bitcast(F32)
                return lambda e: e.matmul(tgt, lhsT=ident[:, 0:1], rhs=ident[:, 0:1], start=True, stop=True)
            return None

        def O(en, meth, reads=(), writes=(), dma=None, **kw):
            return P.op(en, lambda e: getattr(e, meth)(**kw), reads, writes, dma, skip=skip_for(en, None))

        def G(en, calls, reads=(), writes=()):
            return P.group(en, [(lambda e, m=m, kw=kw: getattr(e, m)(**kw)) for m, kw in calls], reads, writes,
                           skip=skip_for(en, calls[0][1].get("out")))

        ring_sem = [P.dsem(f"ring_s{i}") for i in range(NRING)]
        pv_sem, wr_sem, wbd_sem, gf_sem = P.dsem("pv_s"), P.dsem("wr_s"), P.dsem("wbd_s"), P.dsem("gf_s")
        xl_sem = [P.dsem(f"xl{i}") for i in range(NB)]
        out_sem = P.dsem("outs")
        if sparse:
            xsp_sem, y_sem = P.dsem("xsp_s"), P.dsem("y_s")
            h_sem = [P.dsem(f"h_s{i}") for i in range(2)]
            xr_sem = [P.dsem(f"xr_s{i}") for i in range(NT)]
            t2_sem, sc_sem, ix_sem = P.dsem("t2_s"), P.dsem("sc_s"), P.dsem("ix_s")
            g_sem = [P.dsem(f"g_s{i}") for i in range(6)]
            yg_sem = [P.dsem(f"yg_s{i}") for i in range(6)]
        ring_pos = [0]

        def load_w(src_ap, split=False):
            i = ring_pos[0] % NRING
            ring_pos[0] += 1
            dst = ring[i][:].rearrange("p (c h) n -> p c h n", h=2) if split else ring[i][:]
            O("pool", "dma_start", writes=[bf("ring", i)], dma=ring_sem[i], out=dst, in_=src_ap)
            return i

        O("dve", "memset", writes=[bf("junk_a")], ap=junk_a[:], constant=0.0)
        O("dve", "memset", writes=[bf("junk_v")], ap=junk_v[:], constant=0.0)
        O("sp", "dma_start", writes=[bf("pv")], dma=pv_sem, out=pv[:], in_=pv_d)
        for b in range(NB):
            src = x_d[b * 512:(b + 1) * 512, :].rearrange("(i p) d -> p i d", p=128)
            O("sp", "dma_start", writes=[bf("xs", 4 * b + i) for i in range(4)], dma=xl_sem[b],
              out=xs[:, 4 * b:4 * b + 4, :], in_=src)
        O("act", "activation", reads=[bf("pv")], writes=[bf("ident")], out=ident[:], in_=pv[:, PV_ID:PV_ID + 128], func=AF.Copy)
        if 1 in layers:
            O("act", "activation", reads=[bf("pv")], writes=[bf("ident32")], out=ident32[:], in_=pv[:, PV_ID:PV_ID + 128], func=AF.Copy)
            O("sp", "dma_start", writes=[bf("wr_sb")], dma=wr_sem, out=wr_sb[:], in_=wr_d[0].rearrange("(k p) n -> p k n", p=128))
        if sparse:
            O("act", "activation", reads=[bf("pv")], writes=[bf("Ubf")], out=Ubf[:], in_=pv[:, PV_U:PV_U + 128], func=AF.Copy)
            O("dve", "memset", writes=[bf("onesbf")], ap=onesbf[:], constant=1.0)
            for c in range(2):
                O("dve", "tensor_copy", reads=[bf("pv")], writes=[bf("tokid")], out=tokid[:, :, c], in_=pv[:, PV_TOK:PV_TOK + 16])

        def build_wbd(l):
            O("dve", "memset", writes=[bf("wbd")], ap=wbd[:], constant=0.0)
            for g, wd_ in enumerate((wa_d, wx_d)):
                for q in range(2):
                    src = wd_[l].rearrange("(c q) i j -> q i c j", q=2)[q]
                    dst = wbd[q * 64:(q + 1) * 64, g * 4:g * 4 + 4, q * 64:(q + 1) * 64]
                    O("pool", "dma_start", writes=[bf("wbd")], dma=wbd_sem, out=dst, in_=src)

        for l in layers:
            lam = pv[:, l * PV_L + PV_LAM:l * PV_L + PV_LAM + 4]
            c1 = cc[:, l * 4:l * 4 + 4]
            c2 = cc[:, 8 + l * 4:8 + l * 4 + 4]
            O("act", "activation", reads=[bf("pv")], writes=[bf("cc")], out=c1, in_=lam, func=AF.Exp, scale=-1.0)
            O("act", "activation", reads=[bf("cc")], writes=[bf("cc")], out=c1, in_=c1, func=AF.Ln, bias=1.0)
            O("dve", "tensor_scalar", reads=[bf("cc")], writes=[bf("cc")], out=c2, in0=c1, scalar1=-16.0, scalar2=None, op0=ALU.mult)
            O("dve", "tensor_scalar", reads=[bf("cc")], writes=[bf("cc")], out=c1, in0=c1, scalar1=-8.0, scalar2=None, op0=ALU.mult)
            O("dve", "tensor_scalar", reads=[bf("pv")], writes=[bf("hbias")], out=hbias[:, l * 8:l * 8 + 8],
              in0=pv[:, l * PV_L + PV_BA:l * PV_L + PV_BA + 8], scalar1=-1.0, scalar2=None, op0=ALU.mult)

        def pvc(l, base, off=0, n=1):
            c0 = l * PV_L + base + off
            return pv[:, c0:c0 + n]

        def rstd_block(tis):
            t0, t1 = tis[0], tis[-1] + 1
            for ti in tis:
                O("act", "activation", reads=[bf("xs", ti)], writes=[bf("junk"), bf("ss", ti)], out=s16[0][:], in_=xs[:, ti, :],
                  func=AF.Square, accum_out=ss[:, ti:ti + 1])
            sk = [bf("ss", ti) for ti in tis]
            rk = [bf("rs", ti) for ti in tis]
            O("dve", "tensor_scalar", reads=sk, writes=rk, out=rs[:, t0:t1], in0=ss[:, t0:t1], scalar1=1.0 / D, scalar2=EPS,
              op0=ALU.mult, op1=ALU.add)
            O("act", "activation", reads=rk, writes=rk, out=rs[:, t0:t1], in_=rs[:, t0:t1], func=AF.Ln)
            O("act", "activation", reads=rk, writes=rk, out=rs[:, t0:t1], in_=rs[:, t0:t1], func=AF.Exp, scale=-0.5)

        def rstd(ti):
            rstd_block([ti])

        def rmsnorm_T(ti, gcol, dst, dst_c0, dst_buf, have_rstd=False, pb_i=0):
            xb = bf("xs", ti)
            if not have_rstd:
                rstd(ti)
            xn = s16[1 + (ti % 2)]
            xnb = bf("xn", ti % 2)
            O("act", "activation", reads=[xb, bf("rs", ti)], writes=[xnb], out=xn[:], in_=xs[:, ti, :], func=AF.Copy,
              scale=rs[:, ti:ti + 1])
            pb = bf("pst", pb_i)
            G("pe", [("transpose", dict(out=pst[pb_i][:, c * 128:(c + 1) * 128], in_=xn[:, c * 128:(c + 1) * 128], identity=ident[:]))
                     for c in range(8)], reads=[xnb, bf("ident")], writes=[pb])
            gb = gcol.unsqueeze(2).broadcast_to([128, 8, 128])
            src3 = pst[pb_i][:].rearrange("p (c t) -> p c t", c=8)
            O("dve", "tensor_tensor", reads=[pb, bf("pv")], writes=[dst_buf], out=dst[:, :, dst_c0:dst_c0 + 128], in0=src3, in1=gb,
              op=ALU.mult)

        def mm_calls(out_ap, pairs):
            n = len(pairs)
            return [("matmul", dict(out=out_ap, lhsT=lhsT, rhs=rhs, start=(i == 0), stop=(i == n - 1)))
                    for i, (lhsT, rhs) in enumerate(pairs)]

        def router(ti, l):
            if sparse:
                rxn32 = hT[:, 0, :].bitcast(F32)
                rxnT32 = hT[:, 1, :].bitcast(F32).rearrange("p (c t) -> p c t", t=128)
                RXK, RTK = [bf("hTscr", 0)], [bf("hTscr", 1), bf("hTscr", 2)]
                RX0 = RXK + [bf("hT", bb) for bb in range(NB)]
            else:
                rxn32, rxnT32 = xn32, xnT32
                RXK, RTK = [bf("s32", 0), bf("s32", 1)], [bf("s32", 2), bf("s32", 3)]
                RX0 = RXK
            xb = bf("xs", ti)
            O("act", "activation", reads=[xb, bf("rs", ti)], writes=RX0, out=rxn32[:], in_=xs[:, ti, :], func=AF.Copy,
              scale=rs[:, ti:ti + 1])
            yield
            rps = pst[0][:, 0:1024].bitcast(F32)
            RPK = bf("pst", 0)
            for hh in range(2):
                G("pe", [("transpose", dict(out=rps[:, c * 128:(c + 1) * 128],
                                            in_=rxn32[:, (4 * hh + c) * 128:(4 * hh + c + 1) * 128], identity=ident32[:]))
                         for c in range(4)], reads=RXK + [bf("ident32")], writes=[RPK])
                gb = pvc(l, PV_GFFN, 4 * hh, 4).unsqueeze(2).broadcast_to([128, 4, 128])
                O("dve", "tensor_tensor", reads=[RPK, bf("pv")], writes=[RTK[hh]],
                  out=rxnT32[:, 4 * hh:4 * hh + 4, :], in0=rps.rearrange("p (c t) -> p c t", c=4), in1=gb, op=ALU.mult)
                yield
            G("pe", mm_calls(rps[:, 0:NE], [(rxnT32[:, c, :], wr_sb[:, c, :]) for c in range(8)]),
              reads=RTK + [bf("wr_sb")], writes=[RPK])
            lgt = lg[:]
            cbt = comb[:, ti, :]
            O("act", "activation", reads=[RPK], writes=[bf("lg")], out=lgt, in_=rps[:, 0:NE], func=AF.Copy)
            yield
            O("dve", "scalar_tensor_tensor", reads=[bf("lg"), bf("pv")], writes=[bf("lg")], out=lgt, in0=pv[:, PV_IOTA:PV_IOTA + NE],
              scalar=-1e-30, in1=lgt, op0=ALU.mult, op1=ALU.add)
            O("dve", "max", reads=[bf("lg")], writes=[bf("m8")], out=m8[:], in_=lgt)
            O("dve", "tensor_scalar", reads=[bf("lg"), bf("m8")], writes=[bf("comb", ti)], out=cbt, in0=lgt, scalar1=m8[:, 1:2],
              scalar2=None, op0=ALU.is_ge)
            if sparse:
                O("dve", "tensor_copy", reads=[bf("comb", ti)], writes=[bf("msk", ti)], out=msk[:, ti, :], in_=cbt)
                O("dve", "tensor_scalar", reads=[bf("lg"), bf("m8")], writes=[bf("m1", ti)], out=m1[:, ti, :], in0=lgt,
                  scalar1=m8[:, 0:1], scalar2=None, op0=ALU.is_equal)
            O("dve", "tensor_scalar", reads=[bf("m8")], writes=[bf("rsum")], out=rsum[:, 0:1], in0=m8[:, 0:1], scalar1=-1.0,
              scalar2=None, op0=ALU.mult)
            O("act", "activation", reads=[bf("lg"), bf("rsum")], writes=[bf("ex")], out=ex[:], in_=lgt, func=AF.Exp,
              bias=rsum[:, 0:1], scale=1.0)
            O("dve", "tensor_tensor", reads=[bf("comb", ti), bf("ex")], writes=[bf("comb", ti)], out=cbt, in0=cbt, in1=ex[:], op=ALU.mult)
            O("dve", "reduce_sum", reads=[bf("comb", ti)], writes=[bf("rsum")], out=rsum[:, 1:2], in_=cbt, axis=mybir.AxisListType.X)
            O("dve", "reciprocal", reads=[bf("rsum")], writes=[bf("rsum")], out=rsum[:, 1:2], in_=rsum[:, 1:2])
            O("dve", "tensor_scalar", reads=[bf("comb", ti), bf("rsum")], writes=[bf("comb", ti)], out=cbt, in0=cbt,
              scalar1=rsum[:, 1:2], scalar2=None, op0=ALU.mult)
            yield

        def ffn(l):
            experts = [None] if l == 0 else list(range(n_experts))
            items = [(e_, g, ti) for e_ in experts for g in range(ffn_groups) for ti in range(NT)]
            slots = {}

            def stage_gu(n):
                e_, g, ti = items[n]
                if ti == 0:
                    if e_ is None:
                        sg_, su_, sd_ = fg_d[0], fu_d[0], fd_d[0]
                    else:
                        sg_, su_, sd_ = mg_d[0][e_], mu_d[0][e_], md_d[0][e_]
                    a = load_w(sg_[:, g * 512:(g + 1) * 512].rearrange("(k p) n -> p k n", p=128))
                    b_ = load_w(su_[:, g * 512:(g + 1) * 512].rearrange("(k p) n -> p k n", p=128))
                    c_ = load_w(sd_[g * 512:(g + 1) * 512, :].rearrange("(c p) (h n) -> p c h n", p=128, h=2), split=True)
                    slots[(e_, g)] = (a, b_, c_)
                a, b_, c_ = slots[(e_, g)]
                pg, pu = n % 2, 2 + n % 2
                hb = bf("hT", ti // 4)
                calls = []
                for k in range(8):
                    lhsT = hT[:, k, ti * 128:(ti + 1) * 128]
                    calls.append(("matmul", dict(out=psf[pg][:], lhsT=lhsT, rhs=ring[a][:, k, :], start=(k == 0), stop=(k == 7))))
                    calls.append(("matmul", dict(out=psf[pu][:], lhsT=lhsT, rhs=ring[b_][:, k, :], start=(k == 0), stop=(k == 7))))
                G("pe", calls, reads=[hb, bf("ring", a), bf("ring", b_)], writes=[bf("psf", pg), bf("psf", pu)])
                sil = s32[n % 2]
                O("act", "activation", reads=[bf("psf", pg)], writes=[bf("s32", n % 2)], out=sil[:], in_=psf[pg][:], func=AF.Silu)
                O("dve", "tensor_tensor", reads=[bf("s32", n % 2), bf("psf", pu)], writes=[bf(*HIDK[n % 2])], out=hid[n % 2][:, 0:512],
                  in0=sil[:], in1=psf[pu][:], op=ALU.mult)

            def stage_tr(n):
                pt = n % 2
                G("pe", [("transpose", dict(out=pst[pt][:, c * 128:(c + 1) * 128], in_=hid[n % 2][:, c * 128:(c + 1) * 128],
                                            identity=ident[:])) for c in range(4)],
                  reads=[bf(*HIDK[n % 2]), bf("ident")], writes=[bf("pst", pt)])
                O("act", "activation", reads=[bf("pst", pt)], writes=[bf(*k) for k in HIDTK[n % 2]], out=hidT[n % 2][:, 0:512], in_=pst[pt][:, 0:512],
                  func=AF.Copy)

            def stage_dn(n):
                e_, g, ti = items[n]
                c_ = slots[(e_, g)][2]
                xb = bf("xs", ti)
                for h in range(2):
                    bank = 4 + h
                    pairs = [(hidT[n % 2][:, c * 128:(c + 1) * 128], ring[c_][:, 2 * c + h, :]) for c in range(4)]
                    G("pe", mm_calls(psf[bank][:], pairs), reads=[bf(*k) for k in HIDTK[n % 2]] + [bf("ring", c_)], writes=[bf("psf", bank)])
                    xo = xs[:, ti, h * 512:(h + 1) * 512]
                    if e_ is None:
                        O("dve", "tensor_tensor", reads=[bf("psf", bank), xb], writes=[xb], out=xo, in0=psf[bank][:], in1=xo, op=ALU.add)
                    else:
                        O("dve", "scalar_tensor_tensor", reads=[bf("psf", bank), xb, bf("comb", ti)], writes=[xb], out=xo,
                          in0=psf[bank][:], scalar=comb[:, ti, e_:e_ + 1], in1=xo, op0=ALU.mult, op1=ALU.add)

            N = len(items)
            for s in range(N + 2):
                if s < N:
                    stage_gu(s)
                if 0 <= s - 1 < N:
                    stage_tr(s - 1)
                if 0 <= s - 2 < N:
                    stage_dn(s - 2)

        def moe_sparse(l):
            IOA = bass.IndirectOffsetOnAxis
            slots = {}
            for g in range(2):
                a = load_w(mg_d[0][0][:, g * 512:(g + 1) * 512].rearrange("(k p) n -> p k n", p=128))
                b_ = load_w(mu_d[0][0][:, g * 512:(g + 1) * 512].rearrange("(k p) n -> p k n", p=128))
                c_ = load_w(md_d[0][0][g * 512:(g + 1) * 512, :].rearrange("(c p) (h n) -> p c h n", p=128, h=2), split=True)
                slots[(0, g, "m")] = (a, b_, c_)
            P.barrier()
            f2 = lambda t: t[:].rearrange("p i e -> p (i e)")
            v3 = lambda a: a.rearrange("p (i e) -> p i e", e=NE)
            mskf, m1f, combf = f2(msk), f2(m1), f2(comb)
            K2, K3, K4, K5, K6 = (bf("s32", i) for i in (2, 3, 4, 5, 1))
            tot, off, pos, tmp, m2f = (s32[i][:, 0:128] for i in (2, 3, 4, 5, 1))
            allm = [bf("msk", i) for i in range(NT)] + [bf("m1", i) for i in range(NT)] + [bf("comb", i) for i in range(NT)]
            O("act", "activation", reads=allm, writes=[bf("mskb")], out=mskb[:], in_=mskf, func=AF.Copy)
            G("pe", [("matmul", dict(out=psf[0][:, 0:128], lhsT=Ubf[:], rhs=mskb[:], start=True, stop=True))],
              reads=[bf("Ubf"), bf("mskb")], writes=[bf("psf", 0)])
            G("pe", [("matmul", dict(out=psf[1][:, 0:128], lhsT=onesbf[:], rhs=mskb[:], start=True, stop=True))],
              reads=[bf("onesbf"), bf("mskb")], writes=[bf("psf", 1)])
            O("dve", "tensor_copy", reads=[bf("psf", 1)], writes=[K2], out=tot, in_=psf[1][:, 0:128])
            O("dve", "memset", writes=[K3], ap=off[:, 0:NE], constant=0.0)
            for i in range(1, NT):
                O("dve", "tensor_tensor", reads=[K2, K3], writes=[K3], out=off[:, NE * i:NE * i + NE], in0=off[:, NE * (i - 1):NE * i],
                  in1=tot[:, NE * (i - 1):NE * i], op=ALU.add)
            O("dve", "tensor_tensor", reads=[bf("psf", 0), K3], writes=[K4], out=pos, in0=psf[0][:, 0:128], in1=off, op=ALU.add)
            O("dve", "tensor_tensor", reads=[K2, K3], writes=[bf("nef")], out=nef[:], in0=off[:, 120:128], in1=tot[:, 120:128], op=ALU.add)
            O("dve", "tensor_copy", reads=[bf("nef")], writes=[bf("cnt")], out=cnt_i[:], in_=nef[:])
            O("dve", "tensor_tensor", reads=allm, writes=[K6], out=m2f, in0=mskf, in1=m1f, op=ALU.subtract)
            iota3 = pv[:, PV_IOTA:PV_IOTA + NE].unsqueeze(1).broadcast_to([128, NT, NE])
            for c, mc in ((0, m1f), (1, m2f)):
                for q, other in ((0, pos), (1, None), (2, combf)):
                    if other is None:
                        O("dve", "tensor_tensor", reads=allm + [K6, bf("pv")], writes=[K5], out=v3(tmp), in0=v3(mc), in1=iota3, op=ALU.mult)
                    else:
                        O("dve", "tensor_tensor", reads=allm + [K6, K4], writes=[K5], out=tmp, in0=mc, in1=other, op=ALU.mult)
                    O("dve", "reduce_sum", reads=[K5], writes=[bf("PEG")], out=PEG[:, q, 16 * c:16 * c + 16], in_=v3(tmp),
                      axis=mybir.AxisListType.X)
            Pc, Ec, Gc = PEG[:, 0, :], PEG[:, 1, :], PEG[:, 2, :]
            O("dve", "scalar_tensor_tensor", reads=[bf("PEG")], writes=[K5], out=tmp[:, 0:32], in0=Ec, scalar=float(S), in1=Pc,
              op0=ALU.mult, op1=ALU.add)
            O("dve", "tensor_scalar", reads=[K5], writes=[K5], out=tmp[:, 0:32], in0=tmp[:, 0:32], scalar1=float(NE * S - 1), scalar2=0.0,
              op0=ALU.min, op1=ALU.max)
            O("dve", "tensor_copy", reads=[K5], writes=[bf("Ysel")], out=Ysel[:], in_=tmp[:, 0:32])
            Pi, jj = ti32[:, 0, :], ti32[:, 1, :]
            jjf, t0 = tmp[:, 32:64], tmp[:, 64:96]
            O("dve", "tensor_copy", reads=[bf("PEG")], writes=[bf("ti32")], out=Pi, in_=Pc)
            O("dve", "tensor_scalar", reads=[bf("ti32")], writes=[bf("ti32")], out=jj, in0=Pi, scalar1=7, scalar2=None, op0=ALU.arith_shift_right)
            O("dve", "tensor_copy", reads=[bf("ti32")], writes=[K5], out=jjf, in_=jj)
            O("dve", "tensor_scalar", reads=[bf("PEG")], writes=[K5], out=t0, in0=Pc, scalar1=128.0, scalar2=None, op0=ALU.mult)
            O("dve", "scalar_tensor_tensor", reads=[K5], writes=[K5], out=t0, in0=jjf, scalar=-16383.0, in1=t0, op0=ALU.mult, op1=ALU.add)
            O("dve", "scalar_tensor_tensor", reads=[K5, bf("PEG")], writes=[K5], out=t0, in0=Ec, scalar=16.0, in1=t0, op0=ALU.mult, op1=ALU.add)
            O("dve", "tensor_scalar", reads=[K5], writes=[K5], out=t0, in0=t0, scalar1=float(128 * 128 - 1), scalar2=0.0, op0=ALU.min,
              op1=ALU.max)
            O("dve", "tensor_copy", reads=[K5], writes=[bf("Tsc")], out=Tsc[:], in_=t0)
            O("dve", "memset", writes=[bf("idx_all")], ap=idx_all[:], constant=0)
            O("sp", "dma_start", reads=[bf("idx_all")], writes=[bf("T2")], dma=t2_sem, out=T2_d.rearrange("(p a) c -> p (a c)", p=128),
              in_=idx_all[:])
            for c in range(2):
                for i in range(NT):
                    O("pool", "indirect_dma_start", reads=[bf("Tsc"), bf("tokid"), bf("T2")], writes=[bf("T2s", c, i)], dma=sc_sem, out=T2_d,
                      out_offset=IOA(ap=Tsc[:, 16 * c + i:16 * c + i + 1], axis=0), in_=tokid[:, i, :], in_offset=None)
            O("sp", "dma_start", reads=[bf("T2")] + [bf("T2s", c, i) for c in range(2) for i in range(NT)], writes=[bf("idx_all")],
              dma=ix_sem, out=idx_all[:],
              in_=T2_d.rearrange("(p a) c -> p (a c)", p=128))

            gffn3 = pvc(l, PV_GFFN, 0, 8).unsqueeze(2).broadcast_to([128, 8, 128])
            Hall = [bf("H", i) for i in range(NT)]
            npro = [0]

            def guard(e_, j):
                P.guard = (e_ % 2, 128 * j + 1)

            def prologue(e_, j):
                n = npro[0]
                npro[0] += 1
                if j == 0:
                    P.guard = None
                    P.regload(e_ % 2, cnt_i[0:1, e_:e_ + 1], [bf("cnt")])
                kk = bf("s32", 2 + n % 4)
                hg = s32big[:, 2 + n % 4, :].bitcast(BF16)
                col = 2 * (e_ * 16 + j)
                P.guard = None
                O("pool", "indirect_dma_start", reads=Hall + [bf("idx_all")], writes=[kk], dma=g_sem[n % 4], out=hg, out_offset=None,
                  in_=H_d, in_offset=IOA(ap=idx_all[:, col:col + 1], axis=0))
                guard(e_, j)
                pb = bf("pst", n % 2)
                G("pe", [("transpose", dict(out=pst[n % 2][:, c * 128:(c + 1) * 128], in_=hg[:, c * 128:(c + 1) * 128], identity=ident[:]))
                         for c in range(8)], reads=[kk, bf("ident")], writes=[pb])
                O("dve", "tensor_tensor", reads=[pb, bf("pv")], writes=[bf("hTj", j)], out=hT[:, :, j * 128:(j + 1) * 128],
                  in0=pst[n % 2][:].rearrange("p (c t) -> p c t", c=8), in1=gffn3, op=ALU.mult)
                P.guard = None

            def stage_gu(items, n, j0, tag):
                e_, g, j = items[n]
                if j == j0 and (e_, g, tag) not in slots:
                    a = load_w(mg_d[0][e_][:, g * 512:(g + 1) * 512].rearrange("(k p) n -> p k n", p=128))
                    b_ = load_w(mu_d[0][e_][:, g * 512:(g + 1) * 512].rearrange("(k p) n -> p k n", p=128))
                    c_ = load_w(md_d[0][e_][g * 512:(g + 1) * 512, :].rearrange("(c p) (h n) -> p c h n", p=128, h=2), split=True)
                    slots[(e_, g, tag)] = (a, b_, c_)
                a, b_, c_ = slots[(e_, g, tag)]
                pg, pu = n % 2, 2 + n % 2
                guard(e_, j)
                calls = []
                for k in range(8):
                    lhsT = hT[:, k, j * 128:(j + 1) * 128]
                    calls.append(("matmul", dict(out=psf[pg][:], lhsT=lhsT, rhs=ring[a][:, k, :], start=(k == 0), stop=(k == 7))))
                    calls.append(("matmul", dict(out=psf[pu][:], lhsT=lhsT, rhs=ring[b_][:, k, :], start=(k == 0), stop=(k == 7))))
                G("pe", calls, reads=[bf("hTj", j), bf("ring", a), bf("ring", b_)], writes=[bf("psf", pg), bf("psf", pu)])
                sil = s32[n % 2]
                P.guard = None
                O("act", "activation", reads=[bf("psf", pg)], writes=[bf("s32", n % 2)], out=sil[:], in_=psf[pg][:], func=AF.Silu)
                guard(e_, j)
                O("dve", "tensor_tensor", reads=[bf("s32", n % 2), bf("psf", pu)], writes=[bf(*HIDK[n % 2])], out=hid[n % 2][:, 0:512],
                  in0=sil[:], in1=psf[pu][:], op=ALU.mult)
                P.guard = None
                if tag == "m" and g == ffn_groups - 1 and e_ + 1 < n_experts:
                    prologue(e_ + 1, j)

            def stage_tr(items, n, j0, tag):
                e_, g, j = items[n]
                pt = n % 2
                guard(e_, j)
                G("pe", [("transpose", dict(out=pst[pt][:, c * 128:(c + 1) * 128], in_=hid[n % 2][:, c * 128:(c + 1) * 128],
                                            identity=ident[:])) for c in range(4)],
                  reads=[bf(*HIDK[n % 2]), bf("ident")], writes=[bf("pst", pt)])
                P.guard = None
                O("act", "activation", reads=[bf("pst", pt)], writes=[bf(*k) for k in HIDTK[n % 2]], out=hidT[n % 2][:, 0:512], in_=pst[pt][:, 0:512],
                  func=AF.Copy)

            def stage_dn(items, n, j0, tag):
                e_, g, j = items[n]
                c_ = slots[(e_, g, tag)][2]
                xb = bf("xs", j)
                guard(e_, j)
                for h in range(2):
                    bank = 4 + h
                    pairs = [(hidT[n % 2][:, c * 128:(c + 1) * 128], ring[c_][:, 2 * c + h, :]) for c in range(4)]
                    G("pe", mm_calls(psf[bank][:], pairs), reads=[bf(*k) for k in HIDTK[n % 2]] + [bf("ring", c_)], writes=[bf("psf", bank)])
                    xo = xs[:, j, h * 512:(h + 1) * 512]
                    if g == 0:
                        O("dve", "tensor_copy", reads=[bf("psf", bank)], writes=[xb], out=xo, in_=psf[bank][:])
                    else:
                        O("dve", "tensor_tensor", reads=[bf("psf", bank), xb], writes=[xb], out=xo, in0=psf[bank][:], in1=xo, op=ALU.add)
                P.guard = None
                if g == ffn_groups - 1:
                    r0 = e_ * S + j * 128
                    O("sp", "dma_start", reads=[xb], writes=[bf("Y")], dma=y_sem, out=Y_d[r0:r0 + 128, :], in_=xs[:, j, :])

            def run_items(items, j0, tag):
                N = len(items)
                for s_ in range(N + 2):
                    if s_ < N:
                        stage_gu(items, s_, j0, tag)
                    if 0 <= s_ - 1 < N:
                        stage_tr(items, s_ - 1, j0, tag)
                    if 0 <= s_ - 2 < N:
                        stage_dn(items, s_ - 2, j0, tag)

            for j in range(JMAX):
                prologue(0, j)
            run_items([(e_, g, j) for e_ in range(n_experts) for g in range(ffn_groups) for j in range(JMAX)], 0, "m")
            for e_ in range(n_experts):
                P.regload(e_ % 2, cnt_i[0:1, e_:e_ + 1], [bf("cnt")])
                P.region_begin(e_ % 2, 128 * JMAX + 1)
                for j in range(JMAX, NT):
                    prologue(e_, j)
                run_items([(e_, g, j) for g in range(ffn_groups) for j in range(JMAX, NT)], JMAX, "o")
                P.region_end()

            for i in range(NT):
                xb = bf("xs", i)
                O("sp", "dma_start", reads=[bf("Xs", t) for t in range(NT)], writes=[xb], dma=xr_sem[i], out=xs[:, i, :], in_=Xs_d[i * 128:(i + 1) * 128, :])
                for c in range(2):
                    n = 2 * i + c
                    q = n % 6
                    keys = [bf("s32", 2 * q), bf("s32", 2 * q + 1)]
                    yg = s32big[:, 2 * q:2 * q + 2, :].rearrange("p a n -> p (a n)")
                    O("pool", "indirect_dma_start", reads=[bf("Y"), bf("Ysel")], writes=keys, dma=yg_sem[q], out=yg, out_offset=None,
                      in_=Y_d, in_offset=IOA(ap=Ysel[:, 16 * c + i:16 * c + i + 1], axis=0))
                    O("dve", "scalar_tensor_tensor", reads=keys + [xb, bf("PEG")], writes=[xb], out=xs[:, i, :], in0=yg,
                      scalar=PEG[:, 2, 16 * c + i:16 * c + i + 1], in1=xs[:, i, :], op0=ALU.mult, op1=ALU.add)

        for l in layers:
            build_wbd(l)
            O("dve", "memset", writes=[bf("ctail", j) for j in range(4)], ap=ctail[:], constant=0.0)
            O("dve", "memset", writes=[bf("utail", j) for j in range(4)], ap=utail[:], constant=0.0)
            for j in range(4):
                O("dve", "memset", writes=[bf("hlast", j)], ap=hlast[:, j:j + 1], constant=0.0)
            def win_tile(t):
                return load_w(w_in_d[l][:, t * 512:(t + 1) * 512].rearrange("(k p) n -> p k n", p=128))

            def proj(slot_i, oc, bank):
                pairs = [(ring[slot_i][:, k, (oc % 4) * 128:(oc % 4) * 128 + 128], hTm[:, k, :]) for k in range(8)]
                G("pe", mm_calls(psf[bank][:], pairs), reads=[bf("ring", slot_i), bf("hTm")], writes=[bf("psf", bank)])

            def gen_A(b):
                yT_b = bf("yT")
                sc, sv, sbg = win_tile(0), win_tile(2), win_tile(1)
                yield
                for j in range(4):
                    k0, k1 = 0, 1
                    csb, acc = s32[k0], s32[k1]
                    K0, K1 = bf("s32", k0), bf("s32", k1)
                    cvb = bf("cvs")
                    proj(sc, j, 0)
                    yield
                    O("act", "activation", reads=[bf("psf", 0)], writes=[K0], out=csb[:], in_=psf[0][:], func=AF.Copy)
                    proj(sv, 8 + j, 1)
                    yield
                    O("act", "activation", reads=[bf("ctail", j)], writes=[cvb], out=cvs[:, 0:2], in_=ctail[:, j, :], func=AF.Copy)
                    O("dve", "tensor_tensor", reads=[K0, bf("psf", 1)], writes=[cvb], out=cvs[:, 2:514], in0=csb[:],
                      in1=psf[1][:], op=ALU.mult)
                    yield
                    O("dve", "tensor_scalar", reads=[cvb, bf("pv")], writes=[K1], out=acc[:], in0=cvs[:, 0:512],
                      scalar1=pvc(l, PV_CONV, j), scalar2=None, op0=ALU.mult)
                    yield
                    for k in (1, 2):
                        O("dve", "scalar_tensor_tensor", reads=[cvb, K1, bf("pv")], writes=[K1], out=acc[:],
                          in0=cvs[:, k:k + 512], scalar=pvc(l, PV_CONV, k * 4 + j), in1=acc[:], op0=ALU.mult, op1=ALU.add)
                        yield
                    O("act", "activation", reads=[cvb], writes=[bf("ctail", j)], out=ctail[:, j, :], in_=cvs[:, 512:514], func=AF.Copy)
                    proj(sbg, 4 + j, 0)
                    O("dve", "tensor_tensor", reads=[K1, bf("psf", 0)], writes=[yT_b], out=yT[:, j, :], in0=acc[:],
                      in1=psf[0][:], op=ALU.mult)
                    yield

            def gen_B(b, sid, su):
                yT_b = bf("yT")
                ku, kr, ki, ka = (2, 3, 4, 5) if sid == 0 else (8, 9, 10, 11)
                ups = ups_[sid]
                bu, bi = (3, 4) if sid == 0 else (5, 2)
                pu_, pr, pi_ = psf[bu][:], psf[bu][:], psf[bi][:]
                PUK, PRK, PIK = bf("psf", bu), bf("psf", bu), bf("psf", bi)
                KU, KR, KI, KA = bf("s32", ku), bf("s32", kr), bf("s32", ki), bf("s32", ka)
                yield
                for j in (sid, sid + 2):
                    uc, r_, i_, a_ = s32[ku], s32[kr], s32[ki], s32[ka]
                    ucb = s16[3][:, sid * 512:(sid + 1) * 512]
                    UCB = bf("ucb", sid)
                    ub = bf("ups", sid)
                    proj(su, 12 + j, bu)
                    yield
                    O("act", "activation", reads=[bf("utail", j)], writes=[ub], out=ups[:, 0:3], in_=utail[:, j, :], func=AF.Copy)
                    O("act", "activation", reads=[PUK], writes=[ub], out=ups[:, 3:515], in_=pu_, func=AF.Copy)
                    yield
                    O("dve", "tensor_scalar", reads=[ub, bf("pv")], writes=[KU], out=uc[:], in0=ups[:, 0:512],
                      scalar1=pvc(l, PV_LCW, j), scalar2=pvc(l, PV_LCB, j), op0=ALU.mult, op1=ALU.add)
                    yield
                    for k in (1, 2, 3):
                        O("dve", "scalar_tensor_tensor", reads=[ub, KU, bf("pv")], writes=[KU], out=uc[:],
                          in0=ups[:, k:k + 512], scalar=pvc(l, PV_LCW, k * 4 + j), in1=uc[:], op0=ALU.mult, op1=ALU.add)
                        yield
                    O("act", "activation", reads=[ub], writes=[bf("utail", j)], out=utail[:, j, :], in_=ups[:, 512:515], func=AF.Copy)
                    O("act", "activation", reads=[KU], writes=[UCB], out=ucb, in_=uc[:], func=AF.Copy)
                    yield
                    G("pe", [("matmul", dict(out=pr, lhsT=wbd[:, j, :], rhs=ucb, start=True, stop=True))],
                      reads=[bf("wbd"), UCB], writes=[PRK])
                    G("pe", [("matmul", dict(out=pi_, lhsT=wbd[:, 4 + j, :], rhs=ucb, start=True, stop=True))],
                      reads=[bf("wbd"), UCB], writes=[PIK])
                    yield
                    for (t_, ps_, PK_, K_, hb_) in ((r_, pr, PRK, KR, l * 8 + j), (i_, pi_, PIK, KI, l * 8 + 4 + j)):
                        O("act", "activation", reads=[PK_, bf("hbias")], writes=[K_], out=t_[:], in_=ps_,
                          func=AF.Exp, bias=hbias[:, hb_:hb_ + 1], scale=-1.0)
                        yield
                    ri = s32big[:, kr:kr + 2, :].rearrange("p a n -> p (a n)")
                    O("act", "activation", reads=[KR, KI], writes=[KR, KI], out=ri, in_=ri, func=AF.Ln, bias=1.0)
                    yield
                    O("act", "activation", reads=[KR, KI], writes=[KR, KI], out=ri, in_=ri, func=AF.Exp, scale=-1.0)
                    yield
                    O("dve", "tensor_tensor", reads=[KI, KU], writes=[KI], out=i_[:], in0=i_[:], in1=uc[:],
                      op=ALU.mult)
                    O("act", "activation", reads=[KR, bf("cc")], writes=[KA], out=a_[:], in_=r_[:], func=AF.Exp,
                      scale=cc[:, l * 4 + j:l * 4 + j + 1])
                    yield
                    O("act", "activation", reads=[KR, bf("cc")], writes=[KR], out=r_[:], in_=r_[:], func=AF.Exp,
                      scale=cc[:, 8 + l * 4 + j:8 + l * 4 + j + 1])
                    yield
                    O("act", "activation", reads=[KR], writes=[KR], out=r_[:], in_=r_[:], func=AF.Ln, bias=1.0000001, scale=-1.0)
                    yield
                    O("act", "activation", reads=[KR], writes=[KR], out=r_[:], in_=r_[:], func=AF.Exp, scale=0.5)
                    yield
                    O("dve", "tensor_tensor", reads=[KI, KR], writes=[KI], out=i_[:], in0=i_[:], in1=r_[:],
                      op=ALU.mult)
                    yield
                    hlb = bf("hlast", j)
                    O("dve", "tensor_tensor_scan", reads=[KA, KI, hlb], writes=[KU], out=uc[:],
                      data0=a_[:], data1=i_[:], initial=hlast[:, j:j + 1], op0=ALU.mult, op1=ALU.add)
                    yield
                    O("act", "activation", reads=[KU], writes=[hlb], out=hlast[:, j:j + 1], in_=uc[:, 511:512], func=AF.Copy)
                    yield ("wait", ("gelu", b, j))
                    O("dve", "tensor_tensor", reads=[bf("s32", 6 + j % 2), KU], writes=[yT_b], out=yT[:, 4 + j, :],
                      in0=s32[6 + j % 2][:], in1=uc[:], op=ALU.mult)
                    yield ("signal", ("yb", b, j))

            def gen_C(b):
                sg = win_tile(4)
                gps = pst[1][:, 0:1024].bitcast(F32)
                yield
                for j in range(4):
                    qk = 6 + j % 2
                    q_, QK = s32[qk], bf("s32", qk)
                    if j >= 2:
                        yield ("wait", ("yb", b, j - 2))
                    pairs = [(ring[sg][:, k, j * 128:j * 128 + 128], hTm[:, k, :]) for k in range(8)]
                    G("pe", mm_calls(gps, pairs), reads=[bf("ring", sg), bf("hTm")], writes=[bf("pst", 1)])
                    yield
                    O("act", "activation", reads=[bf("pst", 1)], writes=[QK], out=q_[:], in_=gps, func=AF.Square)
                    yield
                    O("dve", "tensor_scalar", reads=[QK], writes=[QK], out=q_[:], in0=q_[:], scalar1=GC1, scalar2=1.0,
                      op0=ALU.mult, op1=ALU.add)
                    yield
                    O("dve", "tensor_tensor", reads=[QK, bf("pst", 1)], writes=[QK], out=q_[:], in0=q_[:], in1=gps, op=ALU.mult)
                    yield
                    O("act", "activation", reads=[QK], writes=[QK], out=q_[:], in_=q_[:], func=AF.Exp, scale=-2.0 * GC0)
                    yield
                    O("act", "activation", reads=[QK], writes=[QK], out=q_[:], in_=q_[:], func=AF.Ln, bias=1.0)
                    yield
                    O("act", "activation", reads=[QK], writes=[QK], out=q_[:], in_=q_[:], func=AF.Exp, scale=-1.0)
                    yield
                    O("dve", "tensor_tensor", reads=[QK, bf("pst", 1)], writes=[QK], out=q_[:], in0=q_[:], in1=gps, op=ALU.mult)
                    yield ("signal", ("gelu", b, j))

            def tail_wout(b):
                yT_b = bf("yT")
                so = [load_w(w_out_d[l][:, h * 512:(h + 1) * 512].rearrange("(k p) n -> p k n", p=128)) for h in range(2)]
                for i in range(4):
                    ti = 4 * b + i
                    xb = bf("xs", ti)
                    for h in range(2):
                        bank = 1 + h
                        pairs = [(yT[:, k, i * 128:(i + 1) * 128], ring[so[h]][:, k, :]) for k in range(8)]
                        G("pe", mm_calls(psf[bank][:], pairs), reads=[yT_b, bf("ring", so[h])], writes=[bf("psf", bank)])
                        xo = xs[:, ti, h * 512:(h + 1) * 512]
                        O("dve", "tensor_tensor", reads=[bf("psf", bank), xb], writes=[xb], out=xo, in0=psf[bank][:], in1=xo, op=ALU.add)

            def gen_tail(b):
                yield
                rstd_block([4 * b + i for i in range(4)])
                yield
                for i in range(4):
                    ti = 4 * b + i
                    if l == 1 and sparse:
                        xb = bf("xs", ti)
                        xn = s16[1 + (ti % 2)]
                        xnb = bf("xn", ti % 2)
                        O("act", "activation", reads=[xb, bf("rs", ti)], writes=[xnb], out=xn[:], in_=xs[:, ti, :], func=AF.Copy,
                          scale=rs[:, ti:ti + 1])
                        O("sp", "dma_start", reads=[xnb], writes=[bf("H", ti)], dma=h_sem[ti % 2], out=H_d[ti * 128:(ti + 1) * 128, :], in_=xn[:])
                        yield
                        yield from router(ti, l)
                        O("sp", "dma_start", reads=[xb], writes=[bf("Xs", ti)], dma=xsp_sem, out=Xs_d[ti * 128:(ti + 1) * 128, :],
                          in_=xs[:, ti, :])
                        yield
                    else:
                        rmsnorm_T(ti, pvc(l, PV_GFFN, 0, 8), hT, ti * 128, bf("hT", b), have_rstd=True)
                        yield
                        if l == 1:
                            yield from router(ti, l)

            def run_streams(gens):
                gens = list(gens)
                blocked, done = {}, set()
                while gens:
                    progressed = False
                    for g_ in list(gens):
                        if id(g_) in blocked:
                            if blocked[id(g_)] not in done:
                                continue
                            del blocked[id(g_)]
                        progressed = True
                        try:
                            r = next(g_)
                        except StopIteration:
                            gens.remove(g_)
                            continue
                        if r is not None:
                            if r[0] == "signal":
                                done.add(r[1])
                            elif r[1] not in done:
                                blocked[id(g_)] = r[1]
                    assert progressed, "stream deadlock"

            for b in range(NB):
                rstd_block([4 * b + i for i in range(4)])
                for i in range(4):
                    rmsnorm_T(4 * b + i, pvc(l, PV_GMIX, 0, 8), hTm, i * 128, bf("hTm"), have_rstd=True, pb_i=i % 2)
                su_ = win_tile(3)
                run_streams([gen_B(b, 0, su_), gen_B(b, 1, su_), gen_C(b), gen_A(b)] + ([gen_tail(b - 1)] if b > 0 else []))
                tail_wout(b)
            run_streams([gen_tail(NB - 1)])
            if l == 1 and sparse:
                moe_sparse(l)
            else:
                ffn(l)

        otoks = []
        if final_norm:
            gfin = xn32
            O("sp", "dma_start", writes=[bf("s32", 0), bf("s32", 1)], dma=gf_sem, out=xn32[:], in_=gfin_d)
        if final_norm:
            rstd_block(list(range(NT)))
        for ti in range(NT):
            xb = bf("xs", ti)
            if final_norm:
                O("dve", "scalar_tensor_tensor", reads=[xb, bf("rs", ti), bf("s32", 0), bf("s32", 1)], writes=[xb], out=xs[:, ti, :], in0=xs[:, ti, :],
                  scalar=rs[:, ti:ti + 1], in1=gfin[:], op0=ALU.mult, op1=ALU.mult)
            otoks.append(O("sp", "dma_start", reads=[xb], dma=out_sem, out=out_d[ti * 128:(ti + 1) * 128, :], in_=xs[:, ti, :]))
        P.wait("sp", otoks)
        P.emit()
    return nc


def pack_pv(inp):
    pv = np.zeros((128, PV_N), np.float32)

    def col(v):
        return np.asarray(v, np.float32).reshape(-1, 128).T

    for l in range(2):
        o = l * PV_L
        pv[:, o + PV_GMIX:o + PV_GMIX + 8] = col(inp["norm_mix"][l])
        pv[:, o + PV_GFFN:o + PV_GFFN + 8] = col(inp["norm_ffn"][l])
        pv[:, o + PV_CONV:o + PV_CONV + 12] = np.asarray(inp["conv_w"][l], np.float32).reshape(3, 4, 128).transpose(2, 0, 1).reshape(128, 12)
        pv[:, o + PV_LCW:o + PV_LCW + 16] = np.asarray(inp["lru_conv_w"][l], np.float32).reshape(4, 4, 128).transpose(2, 0, 1).reshape(128, 16)
        pv[:, o + PV_LCB:o + PV_LCB + 4] = col(inp["lru_conv_b"][l])
        pv[:, o + PV_BA:o + PV_BA + 4] = col(inp["lru_ba"][l])
        pv[:, o + PV_BX:o + PV_BX + 4] = col(inp["lru_bx"][l])
        pv[:, o + PV_LAM:o + PV_LAM + 4] = col(inp["lru_lambda"][l])
    pv[:, PV_GFIN:PV_GFIN + 8] = col(inp["norm_final"])
    pv[:, PV_ID:PV_ID + 128] = np.eye(128, dtype=np.float32)
    pv[:, PV_U:PV_U + 128] = np.triu(np.ones((128, 128), np.float32), k=1)
    pv[:, PV_TOK:PV_TOK + 16] = (128.0 * np.arange(16)[None, :] + np.arange(128)[:, None]).astype(np.float32)
    pv[:, PV_IOTA:PV_IOTA + 8] = np.arange(8, dtype=np.float32)[None, :]
    return pv


_W0 = ("ffn_w_gate", "ffn_w_up", "ffn_w_down")
_W1 = ("w_router", "moe_w_gate", "moe_w_up", "moe_w_down")
_WC = ("w_in", "w_out", "lru_wa", "lru_wx")
FUSED = True


def _launch(nc, x, inp, names, pv, gfin):
    n = x.shape[0]
    shared = {k: np.ascontiguousarray(np.asarray(inp[k], np.float32)) for k in names}
    in_maps = []
    for c in range(n):
        m = {"x": np.ascontiguousarray(x[c]), "pv": pv, "gfin": gfin}
        m.update(shared)
        in_maps.append(m)
    res = run_bass_kernel_spmd(nc, in_maps, core_ids=list(range(n)))
    return np.stack([r["out"] for r in res.results], axis=0)


def kernel(**inputs):
    x = np.asarray(inputs["x"], np.float32)
    pv = pack_pv(inputs)
    gfin = np.ascontiguousarray(np.broadcast_to(np.asarray(inputs["norm_final"], np.float32)[None, :], (128, D)))
    if FUSED:
        nc = build(layers=(0, 1), final_norm=True)
        return _launch(nc, x, inputs, _WC + _W0 + _W1, pv, gfin)
    nc0 = build(layers=(0,), final_norm=False)
    x1 = _launch(nc0, x, inputs, _WC + _W0, pv, gfin)
    nc1 = build(layers=(1,), final_norm=True)
    return _launch(nc1, x1, inputs, _WC + _W1, pv, gfin)
```
